# Optimizing a Trainium2 kernel written in Bass

```python
import jax, jax.numpy as jnp
from jax import lax
import numpy as np

D_MODEL = 1024
BATCH = 16
SEQ = 2048
DEPTH = 2

MEM_LEN = 256
Q_BLOCK = 128
ROPE_THETA = 10000.0
NORM_EPS = 1e-6
FOX_HEADS = 6
FOX_DIM = 64
FORGET_BIAS_MEAN = 2.0
MLA_HEADS = 6
MLA_NOPE = 64
MLA_ROPE = 32
MLA_V = 64
MLA_Q_RANK = 256
MLA_KV_RANK = 128
DSA_HEADS = 4
DSA_DIM = 64
IDX_HEADS = 8
IDX_DIM = 32
TOPK_MAX = 256
XA_HEADS = 4
XA_DIM = 128
D_FF = 2816
IN_SIZES = (FOX_HEADS * FOX_DIM, FOX_HEADS * FOX_DIM, FOX_HEADS * FOX_DIM, FOX_HEADS,
            MLA_Q_RANK, MLA_KV_RANK, MLA_ROPE,
            DSA_HEADS * DSA_DIM, DSA_DIM, DSA_DIM, IDX_HEADS * IDX_DIM, IDX_DIM, IDX_HEADS)
D_IN = sum(IN_SIZES)
D_MIX = FOX_HEADS * FOX_DIM + MLA_HEADS * MLA_V + DSA_HEADS * DSA_DIM

kernel_name = "hybrid_fox_mla_dsa_macaron_block"


def _rmsnorm(x, g):
    xf = x.astype(jnp.float32)
    y = xf * lax.rsqrt(jnp.mean(xf * xf, axis=-1, keepdims=True) + NORM_EPS)
    return (y * g.astype(jnp.float32)).astype(x.dtype)


def _rope(x, pos):
    half = x.shape[-1] // 2
    inv = ROPE_THETA ** (-jnp.arange(half, dtype=jnp.float32) / half)
    ang = pos.astype(jnp.float32)[:, None] * inv[None, :]
    cos = jnp.cos(ang)[:, None, :]
    sin = jnp.sin(ang)[:, None, :]
    xf = x.astype(jnp.float32)
    x1, x2 = xf[..., :half], xf[..., half:]
    return jnp.concatenate([x1 * cos - x2 * sin, x2 * cos + x1 * sin], axis=-1).astype(x.dtype)


def _swiglu(h, wi, wo):
    gate, up = jnp.split(h @ wi, 2, axis=-1)
    return (jax.nn.silu(gate) * up) @ wo


def _split(a, sizes):
    offsets = [int(o) for o in np.cumsum(sizes)[:-1]]
    return jnp.split(a, offsets, axis=-1)


def _to_blocks(a):
    b, s = a.shape[0], a.shape[1]
    a = a.reshape((b, s // Q_BLOCK, Q_BLOCK) + a.shape[2:])
    return jnp.moveaxis(a, 1, 0)


def _from_blocks(a):
    a = jnp.moveaxis(a, 0, 1)
    return a.reshape((a.shape[0], a.shape[1] * a.shape[2]) + a.shape[3:])


def _causal_block_attention(q, k, v, cum_logf=None):
    s_len = q.shape[1]
    nb = s_len // Q_BLOCK
    scale = q.shape[-1] ** -0.5
    pos = jnp.arange(s_len)
    fk = None if cum_logf is None else jnp.swapaxes(cum_logf, 1, 2)

    def one_block(args):
        qb, pb, fb = args
        s = jnp.einsum('bqhd,bkhd->bhqk', qb, k).astype(jnp.float32) * scale
        if fk is not None:
            s = s + (jnp.swapaxes(fb, 1, 2)[..., :, None] - fk[:, :, None, :])
        s = jnp.where((pb[:, None] >= pos[None, :])[None, None], s, -jnp.inf)
        p = jax.nn.softmax(s, axis=-1).astype(v.dtype)
        return jnp.einsum('bhqk,bkhd->bqhd', p, v)

    fq = None if cum_logf is None else _to_blocks(cum_logf)
    out = lax.map(one_block, (_to_blocks(q), pos.reshape(nb, Q_BLOCK), fq))
    return _from_blocks(out)


def _dsa_attention(q, k, v, qi, ki, wi, n_sel):
    s_len = q.shape[1]
    nb = s_len // Q_BLOCK
    scale = q.shape[-1] ** -0.5
    pos = jnp.arange(s_len)
    gather = jax.vmap(lambda t, i: t[i])

    def one_block(args):
        qb, qib, wb, pb = args
        sc = jnp.einsum('bqhd,bkd->bqhk', qib, ki).astype(jnp.float32) * (IDX_DIM ** -0.5)
        iscore = jnp.einsum('bqhk,bqh->bqk', jax.nn.relu(sc), wb.astype(jnp.float32))
        iscore = jnp.where((pb[:, None] >= pos[None, :])[None], iscore, -jnp.inf)
        _, sel = lax.top_k(iscore, n_sel)
        valid = sel <= pb[None, :, None]
        k_sel = gather(k, sel)
        v_sel = gather(v, sel)
        s = jnp.einsum('bqhd,bqkd->bqhk', qb, k_sel).astype(jnp.float32) * scale
        s = jnp.where(valid[:, :, None, :], s, -jnp.inf)
        p = jax.nn.softmax(s, axis=-1).astype(v.dtype)
        return jnp.einsum('bqhk,bqkd->bqhd', p, v_sel)

    out = lax.map(one_block, (_to_blocks(q), _to_blocks(qi), _to_blocks(wi),
                              pos.reshape(nb, Q_BLOCK)))
    return _from_blocks(out)


def setup_inputs(seed: int = 0) -> dict:
    key = jax.random.key(seed)
    ks = iter(jax.random.split(key, 32))
    f32 = jnp.float32

    def w(shape, fan_in):
        return jax.random.normal(next(ks), shape, f32) * (fan_in ** -0.5)

    def gain(shape):
        return 1.0 + 0.05 * jax.random.normal(next(ks), shape, f32)

    L = DEPTH
    return {
        'x': jax.random.normal(next(ks), (BATCH, SEQ, D_MODEL), f32),
        'mem': jax.random.normal(next(ks), (BATCH, MEM_LEN, D_MODEL), f32),
        'ffn1_norm': gain((L, D_MODEL)),
        'ffn1_wi': w((L, D_MODEL, 2 * D_FF), D_MODEL),
        'ffn1_wo': w((L, D_FF, D_MODEL), D_FF),
        'mix_norm': gain((L, D_MODEL)),
        'w_in': w((L, D_MODEL, D_IN), D_MODEL),
        'b_forget': FORGET_BIAS_MEAN + 0.1 * jax.random.normal(next(ks), (L, FOX_HEADS), f32),
        'mla_q_norm': gain((L, MLA_Q_RANK)),
        'mla_w_uq': w((L, MLA_Q_RANK, MLA_HEADS * (MLA_NOPE + MLA_ROPE)), MLA_Q_RANK),
        'mla_kv_norm': gain((L, MLA_KV_RANK)),
        'mla_w_ukv': w((L, MLA_KV_RANK, MLA_HEADS * (MLA_NOPE + MLA_V)), MLA_KV_RANK),
        'w_out': w((L, D_MIX, D_MODEL), D_MIX),
        'xa_norm': gain((L, D_MODEL)),
        'mem_norm': gain((L, D_MODEL)),
        'xa_wq': w((L, D_MODEL, XA_HEADS * XA_DIM), D_MODEL),
        'xa_wkv': w((L, D_MODEL, 2 * XA_HEADS * XA_DIM), D_MODEL),
        'xa_wo': w((L, XA_HEADS * XA_DIM, D_MODEL), XA_HEADS * XA_DIM),
        'ffn2_norm': gain((L, D_MODEL)),
        'ffn2_wi': w((L, D_MODEL, 2 * D_FF), D_MODEL),
        'ffn2_wo': w((L, D_FF, D_MODEL), D_FF),
        'final_norm': gain((D_MODEL,)),
    }


def reference(x, mem, ffn1_norm, ffn1_wi, ffn1_wo, mix_norm, w_in, b_forget,
              mla_q_norm, mla_w_uq, mla_kv_norm, mla_w_ukv, w_out,
              xa_norm, mem_norm, xa_wq, xa_wkv, xa_wo,
              ffn2_norm, ffn2_wi, ffn2_wo, final_norm):
    b, s_len, _ = x.shape
    m_len = mem.shape[1]
    pos = jnp.arange(s_len)
    n_sel = min(TOPK_MAX, s_len // 4)

    for l in range(DEPTH):
        x = x + 0.5 * _swiglu(_rmsnorm(x, ffn1_norm[l]), ffn1_wi[l], ffn1_wo[l])

        h = _rmsnorm(x, mix_norm[l])
        (fq, fk, fv, ff, mcq, mckv, mkr,
         dq, dk, dv, dqi, dki, dwi) = _split(h @ w_in[l], IN_SIZES)

        log_f = jax.nn.log_sigmoid((ff + b_forget[l]).astype(jnp.float32))
        cum_logf = jnp.cumsum(log_f, axis=1)
        a_out = _causal_block_attention(
            fq.reshape(b, s_len, FOX_HEADS, FOX_DIM),
            fk.reshape(b, s_len, FOX_HEADS, FOX_DIM),
            fv.reshape(b, s_len, FOX_HEADS, FOX_DIM), cum_logf)

        qm = (_rmsnorm(mcq, mla_q_norm[l]) @ mla_w_uq[l]).reshape(
            b, s_len, MLA_HEADS, MLA_NOPE + MLA_ROPE)
        q_nope, q_rope = qm[..., :MLA_NOPE], qm[..., MLA_NOPE:]
        kvm = (_rmsnorm(mckv, mla_kv_norm[l]) @ mla_w_ukv[l]).reshape(
            b, s_len, MLA_HEADS, MLA_NOPE + MLA_V)
        k_nope, v_m = kvm[..., :MLA_NOPE], kvm[..., MLA_NOPE:]
        k_rope = _rope(mkr.reshape(b, s_len, 1, MLA_ROPE), pos)
        q_m = jnp.concatenate([q_nope, _rope(q_rope, pos)], axis=-1)
        k_m = jnp.concatenate(
            [k_nope, jnp.broadcast_to(k_rope, (b, s_len, MLA_HEADS, MLA_ROPE))], axis=-1)
        b_out = _causal_block_attention(q_m, k_m, v_m)

        q_c = _rope(dq.reshape(b, s_len, DSA_HEADS, DSA_DIM), pos)
        k_c = _rope(dk.reshape(b, s_len, 1, DSA_DIM), pos)[:, :, 0]
        qi = _rope(dqi.reshape(b, s_len, IDX_HEADS, IDX_DIM), pos)
        ki = _rope(dki.reshape(b, s_len, 1, IDX_DIM), pos)[:, :, 0]
        wi = dwi * (IDX_HEADS ** -0.5)
        c_out = _dsa_attention(q_c, k_c, dv, qi, ki, wi, n_sel)

        mixed = jnp.concatenate([a_out.reshape(b, s_len, -1), b_out.reshape(b, s_len, -1),
                                 c_out.reshape(b, s_len, -1)], axis=-1)
        x = x + mixed @ w_out[l]

        hq = _rmsnorm(x, xa_norm[l])
        hm = _rmsnorm(mem, mem_norm[l])
        qx = (hq @ xa_wq[l]).reshape(b, s_len, XA_HEADS, XA_DIM)
        kx, vx = jnp.split((hm @ xa_wkv[l]).reshape(b, m_len, 2 * XA_HEADS, XA_DIM), 2, axis=2)
        sx = jnp.einsum('bqhd,bmhd->bhqm', qx, kx).astype(jnp.float32) * (XA_DIM ** -0.5)
        px = jax.nn.softmax(sx, axis=-1).astype(vx.dtype)
        ox = jnp.einsum('bhqm,bmhd->bqhd', px, vx).reshape(b, s_len, XA_HEADS * XA_DIM)
        x = x + ox @ xa_wo[l]

        x = x + 0.5 * _swiglu(_rmsnorm(x, ffn2_norm[l]), ffn2_wi[l], ffn2_wo[l])

    return _rmsnorm(x, final_norm)
```

```python
import numpy as np
from contextlib import ExitStack
import concourse.bass as bass
import concourse.mybir as mybir
from concourse.bass_utils import run_bass_kernel_spmd

F32 = mybir.dt.float32
BF16 = mybir.dt.bfloat16
AF = mybir.ActivationFunctionType
ALU = mybir.AluOpType

D = 1024
T = 2048
L = 2
NSEQ = 2
DFF = 2816
NF = 22
MEM = 256
TC = 512
NTC = T // TC
EPS = 1e-6
NEG = -30000.0
BIGNEG = -1.0e30
ENG = ['pe', 'act', 'dve', 'pool', 'sp']

O_FQ, O_FK, O_FV, O_FF = 0, 384, 768, 1152
O_CQ, O_CKV, O_KR = 1158, 1414, 1542
O_DQ, O_DK, O_DV, O_DQI, O_DKI, O_DWI = 1574, 1830, 1894, 1958, 2214, 2246
X_KR, X_DQ, X_DK, X_DQI, X_KI, X_KIS = 0, 32, 288, 352, 608, 704
NX = 800


class Tok:
    __slots__ = ('w', 'r')

    def __init__(self):
        self.w = None
        self.r = {}


class Prog:
    def __init__(self, nc, es):
        self.nc = nc
        self.es = es
        self.semh = {e: es.enter_context(nc.semaphore('s_' + e)) for e in ENG}
        self.cnt = {e: 0 for e in ENG}
        self.seen = {e: {} for e in ENG}
        self.q = {e: [] for e in ENG}
        self.clock = {e: [None] for e in ENG}
        self.toks = {}
        self.dcnt = {}
        self.nwait = 0

    def tok(self, *key):
        t = self.toks.get(key)
        if t is None:
            t = self.toks[key] = Tok()
        return t

    def stream(self, name):
        if name not in self.semh:
            self.semh[name] = self.es.enter_context(self.nc.semaphore('d_' + name))
            self.dcnt[name] = 0
        return name

    def need(self, eng, ev, raw):
        if ev is None:
            return
        key, val = ev
        if key == eng and (eng == 'pe' or eng == 'sp' or not raw):
            return
        sn = self.seen[eng]
        if sn.get(key, 0) >= val:
            return
        sem = self.semh[key]
        self.q[eng].append(lambda e, sem=sem, val=val: e.wait_ge(sem, val))
        self.nwait += 1
        sn[key] = val
        if key in self.clock and val < len(self.clock[key]):
            for k2, v2 in self.clock[key][val].items():
                if sn.get(k2, 0) < v2:
                    sn[k2] = v2

    def _deps(self, eng, reads, writes):
        for t in reads:
            self.need(eng, t.w, True)
        for t in writes:
            self.need(eng, t.w, False)
            for k, v in t.r.items():
                self.need(eng, (k, v), False)

    def _mark(self, ev, reads, writes):
        k, v = ev
        for t in reads:
            if t.r.get(k, 0) < v:
                t.r[k] = v
        for t in writes:
            t.w = ev
            t.r = {}

    def op(self, eng, fn, reads=(), writes=(), inc=True):
        self._deps(eng, reads, writes)
        if inc:
            self.cnt[eng] += 1
            ev = (eng, self.cnt[eng])
            sem = self.semh[eng]
            self.q[eng].append(lambda e, fn=fn, sem=sem: fn(e).then_inc(sem, 1))
            self.clock[eng].append(dict(self.seen[eng]))
        else:
            ev = (eng, self.cnt[eng] + 1)
            self.q[eng].append(lambda e, fn=fn: fn(e))
        self._mark(ev, reads, writes)

    def dma(self, eng, stream, out, in_, reads=(), writes=()):
        self.stream(stream)
        self._deps(eng, reads, writes)
        self.dcnt[stream] += 16
        ev = (stream, self.dcnt[stream])
        sem = self.semh[stream]
        self.q[eng].append(lambda e, out=out, in_=in_, sem=sem: e.dma_start(out=out, in_=in_).then_inc(sem, 16))
        self._mark(ev, reads, writes)
        return ev

    def act(self, out, in_, func, reads, writes, bias=None, scale=None):
        kw = {}
        if bias is not None:
            kw['bias'] = bias
        if scale is not None:
            kw['scale'] = scale
        self.op('act', lambda e: e.activation(out=out, in_=in_, func=func, **kw), reads, writes)

    def stt(self, eng, out, in0, scalar, in1, op0, op1, reads, writes):
        self.op(eng, lambda e: e.scalar_tensor_tensor(out=out, in0=in0, scalar=scalar, in1=in1, op0=op0, op1=op1),
                reads, writes)

    def tt(self, eng, out, in0, in1, op, reads, writes):
        self.op(eng, lambda e: e.tensor_tensor(out=out, in0=in0, in1=in1, op=op), reads, writes)

    def ts(self, eng, out, in0, s1, s2, op0, op1, reads, writes, accum_out=None):
        kw = {}
        if accum_out is not None:
            kw['accum_out'] = accum_out
        if op1 is None:
            self.op(eng, lambda e: e.tensor_scalar(out=out, in0=in0, scalar1=s1, scalar2=None, op0=op0, **kw),
                    reads, writes)
        else:
            self.op(eng, lambda e: e.tensor_scalar(out=out, in0=in0, scalar1=s1, scalar2=s2, op0=op0, op1=op1, **kw),
                    reads, writes)

    def recip(self, eng, out, in_, reads, writes):
        self.op(eng, lambda e: e.reciprocal(out=out, in_=in_), reads, writes)

    def copy(self, eng, out, in_, reads, writes):
        if eng == 'act':
            self.op(eng, lambda e: e.copy(out=out, in_=in_), reads, writes)
        else:
            self.op(eng, lambda e: e.tensor_copy(out=out, in_=in_), reads, writes)

    def memset(self, eng, out, val, reads, writes):
        self.op(eng, lambda e: e.memset(out, val), reads, writes)

    def barrier(self):
        for e in ['pe', 'act', 'dve', 'pool', 'sp']:
            for o in ['pe', 'act', 'dve', 'pool']:
                if o != e and self.cnt[o] > 0:
                    self.need(e, (o, self.cnt[o]), True)
            for st, v in self.dcnt.items():
                if v > 0 and not (st.startswith('wi') or st.startswith('wo')):
                    self.need(e, (st, v), True)

    def emit(self):
        nc = self.nc
        q = self.q
        with nc.Block() as block:
            @block.tensor
            def _(e):
                for f in q['pe']:
                    f(e)

            @block.scalar
            def _(e):
                for f in q['act']:
                    f(e)

            @block.vector
            def _(e):
                for f in q['dve']:
                    f(e)

            @block.gpsimd
            def _(e):
                for f in q['pool']:
                    f(e)

            @block.sync
            def _(e):
                for f in q['sp']:
                    f(e)


class Ring:
    def __init__(self, P, name, tiles):
        self.P = P
        self.name = name
        self.tiles = tiles
        self.i = 0

    def next(self):
        k = self.i % len(self.tiles)
        self.i += 1
        return self.tiles[k], self.P.tok(self.name, k), '%s%d' % (self.name, k)


def build(nc, cfg):
    nseq = cfg.get('nseq', NSEQ)
    layers = cfg.get('layers', L)
    phases = cfg.get('phases', {'ffn1', 'mix', 'xa', 'ffn2', 'final'})
    es = ExitStack()
    P = Prog(nc, es)

    used_inputs = cfg.get('inputs', None)

    def din(name, shape, dt=F32):
        if used_inputs is not None and name not in used_inputs:
            return None
        return nc.dram_tensor(name, list(shape), dt, kind="ExternalInput").ap()

    xT_d = din('xT', [nseq, D, T])
    memT_d = din('memT', [nseq, D, MEM])
    wi_d = [din('ffn1_wi', [L, D, 2 * DFF]), din('ffn2_wi', [L, D, 2 * DFF])]
    wo_d = [din('ffn1_wo', [L, DFF, D]), din('ffn2_wo', [L, DFF, D])]
    win_d = din('w_in', [L, D, 2254])
    winx_d = din('w_inx', [L, D, NX])
    wuq_d = din('mla_w_uq', [L, 256, 576])
    wuqs_d = din('mla_w_uqs', [L, 256, 576])
    wukv_d = din('mla_w_ukv', [L, 128, 768])
    wout_d = din('w_out', [L, D, D])
    xwq_d = din('xa_wq', [L, D, 512])
    xwkv_d = din('xa_wkv', [L, D, 1024])
    xwo_d = din('xa_wo', [L, 512, D])
    gains_d = din('gains', [128, 96])
    small_d = din('small', [128, 16])
    tab_d = din('tables', [4, 128, T])
    cmask_d = din('cmask', [128, 4 * TC])
    ident_d = din('ident4', [128, 4 * 128])
    cts_d = din('causal_ts', [128, 128])
    out_d = nc.dram_tensor('outT', [nseq, D, T], F32, kind="ExternalOutput").ap()

    def sb(name, shape, dt):
        return es.enter_context(nc.sbuf_tensor('sb_' + name, list(shape), dt))

    def ps(name):
        return es.enter_context(nc.psum_tensor(name, [128, TC], F32))

    xT = sb('xT', [128, 8, T], F32)
    hT = sb('hT', [128, 8, T], BF16)
    ident4 = sb('ident4', [128, 4, 128], BF16)
    cmask = sb('cmask', [128, 4, TC], BF16)
    ones_bf = sb('ones_bf', [128, 128], BF16)
    cts = sb('cts', [128, 128], F32)
    gains = sb('gains', [128, 96], F32)
    small = sb('small', [128, 16], F32)
    nbf = sb('nbf', [128, 2], F32)
    wi_tiles = [sb('wiring%d' % i, [128, 8, 256], BF16) for i in range(3)]
    wo_tiles = [sb('woring%d' % i, [128, 11, 128], BF16) for i in range(2)]
    wi_ring = Ring(P, 'wi', wi_tiles)
    wo_ring = Ring(P, 'wo', wo_tiles)
    SCR_BYTES = 90112
    scr = sb('scr', [128, SCR_BYTES // 4], F32)

    def view(off, shape, dt):
        n = int(np.prod(shape))
        esz = 4 if dt == F32 else 2
        assert off % 4 == 0 and (n * esz) % 4 == 0 and off + n * esz <= SCR_BYTES, (off, shape)
        a = scr[:, off // 4: off // 4 + (n * esz) // 4]
        if dt != F32:
            a = a.bitcast(dt)
        if len(shape) == 2:
            a = a.rearrange("p (a b) -> p a b", a=shape[0])
        elif len(shape) == 3:
            a = a.rearrange("p (a b c) -> p a b c", a=shape[0], b=shape[1])
        return a

    banks = [ps('bank%d' % i) for i in range(8)]
    psS = Ring(P, 'psS', banks[0:4])
    psA = Ring(P, 'psA', banks[4:7])
    psM = Ring(P, 'psM', banks[7:8])

    def mm(out, lhsT, rhs, start, stop, reads, writes, inc):
        P.op('pe', lambda e: e.matmul(out, lhsT, rhs, start=start, stop=stop), reads=reads, writes=writes, inc=inc)

    def load_w(ring, dram_ap, ncols_total=None, parts=None):
        tile, tk, st = ring.next()
        for i, (dst_fn, src) in enumerate(parts):
            P.dma('pool', st, dst_fn(tile), src, writes=[tk])
        return tile, tk

    def gain_ap(idx, c):
        return gains[:, idx * 8 + c: idx * 8 + c + 1]

    def norm_chunk(srcs, nfeat, gain_fn, dsts, n, nt_off):
        sq = view(nt_off, [8, TC], BF16)
        s_t = view(nt_off + 8192, [TC], F32)
        r_t = view(nt_off + 10240, [TC], F32)
        nch = len(srcs)
        for c, (sap, stk) in enumerate(srcs):
            P.act(sq[:, c, 0:n], sap, AF.Square, [stk], [P.tok('nt_sq', c)])
        bank, btk, _ = psM.next()
        for c in range(nch):
            mm(bank[:, 0:n], ones_bf[:, :], sq[:, c, 0:n], c == 0, c == nch - 1,
               [P.tok('nt_sq', c)] + CT, [btk], c == nch - 1)
        P.act(s_t[:, 0:n], bank[:, 0:n], AF.Sqrt, [btk], [P.tok('nt_s')], bias=eps_t[:, 0:1], scale=1.0 / nfeat)
        P.recip('dve', r_t[:, 0:n], s_t[:, 0:n], [P.tok('nt_s')], [P.tok('nt_r')])
        for c, ((sap, stk), (dap, dtk)) in enumerate(zip(srcs, dsts)):
            P.stt('dve', dap, sap, gain_fn(c), r_t[:, 0:n], ALU.mult, ALU.mult, [stk, P.tok('nt_r')], [dtk])

    def norm_x(gidx, nt_off):
        for tc in range(NTC):
            ts = slice(tc * TC, (tc + 1) * TC)
            norm_chunk([(xT[:, c, ts], xtok(c, tc)) for c in range(8)], D, lambda c: gain_ap(gidx, c),
                       [(hT[:, c, ts], htok(c, tc)) for c in range(8)], TC, nt_off)

    eps_t = sb('eps_t', [128, 1], F32)

    P.dma('sp', 'c0', gains[:, :], gains_d, writes=[P.tok('gains')])
    P.dma('sp', 'c0', small[:, :], small_d, writes=[P.tok('gains')])
    P.dma('sp', 'c0', cts[:, :], cts_d, writes=[P.tok('gains')])
    P.dma('pool', 'c1', ident4[:, :, :], ident_d.rearrange("p (a b) -> p a b", a=4), writes=[P.tok('consts')])
    P.dma('pool', 'c1', cmask[:, :, :], cmask_d.rearrange("p (a b) -> p a b", a=4), writes=[P.tok('consts')])
    P.memset('dve', ones_bf[:, :], 1.0, [], [P.tok('consts2')])
    P.memset('dve', eps_t[:, :], EPS, [], [P.tok('consts2')])
    P.ts('dve', nbf[:, 0:2], small[:, 8:10], -1.0, None, ALU.mult, None, [P.tok('gains')], [P.tok('nbf')])
    CT = [P.tok('gains'), P.tok('consts'), P.tok('consts2')]

    def xtok(c, tc):
        return P.tok('x', c, tc)

    def htok(c, tc):
        return P.tok('h', c, tc)

    def ffn(l, which, gidx):
        wi = wi_d[which][l]
        wo = wo_d[which][l]
        A_OFF = 0
        actT = view(A_OFF, [11, T], BF16)
        NT_OFF = 45056
        sg_t = [view(57344 + 2048 * i, [TC], F32) for i in range(2)]
        norm_x(gidx, NT_OFF)
        wi_v = wi.rearrange("(kc p) n -> p kc n", p=128)
        wo_v = wo.rearrange("(fc p) n -> p fc n", p=128)
        sgi = 0
        for fh in range(2):
            for f in range(11):
                fg = fh * 11 + f
                wt, wtk = load_w(wi_ring, None, parts=[
                    (lambda t: t[:, :, 0:128], wi_v[:, :, fg * 128:(fg + 1) * 128]),
                    (lambda t: t[:, :, 128:256], wi_v[:, :, DFF + fg * 128: DFF + (fg + 1) * 128])])
                for tc in range(NTC):
                    ts = slice(tc * TC, (tc + 1) * TC)
                    gb, gtk, _ = psS.next()
                    ub, utk, _ = psS.next()
                    for kc in range(8):
                        mm(gb[:, :], wt[:, kc, 0:128], hT[:, kc, ts], kc == 0, kc == 7,
                           [wtk, htok(kc, tc)] + CT, [gtk], kc == 7)
                    for kc in range(8):
                        mm(ub[:, :], wt[:, kc, 128:256], hT[:, kc, ts], kc == 0, kc == 7,
                           [wtk, htok(kc, tc)], [utk], kc == 7)
                    sg = sg_t[sgi % 2]
                    sgk = P.tok('sg', sgi % 2)
                    sgi += 1
                    P.act(sg[:, :], gb[:, :], AF.Silu, [gtk], [sgk])
                    P.tt('dve', actT[:, f, ts], sg[:, :], ub[:, :], ALU.mult, [sgk, utk], [P.tok('act', f, tc)])
            for m in range(8):
                wt, wtk = load_w(wo_ring, None, parts=[
                    (lambda t: t[:, :, :], wo_v[:, fh * 11:(fh + 1) * 11, m * 128:(m + 1) * 128])])
                for tc in range(NTC):
                    ts = slice(tc * TC, (tc + 1) * TC)
                    ob, otk, _ = psA.next()
                    for f in range(11):
                        mm(ob[:, :], wt[:, f, :], actT[:, f, ts], f == 0, f == 10,
                           [wtk, P.tok('act', f, tc)], [otk], f == 10)
                    P.stt('dve', xT[:, m, ts], ob[:, :], 0.5, xT[:, m, ts], ALU.mult, ALU.add,
                          [otk, xtok(m, tc)], [xtok(m, tc)])
        P.barrier()


    def wslot(parts):
        tile, tk, st = wi_ring.next()
        for (k0, k1, c0, c1, src) in parts:
            P.dma('pool', st, tile[:, k0:k1, c0:c1], src, writes=[tk])
        return tile, tk

    def fm(w2d):
        return w2d.rearrange("(kc p) n -> p kc n", p=128)

    def proj(wt, wtk, c0, M, src_fn, nkc, tc, n=TC, ring=None):
        bank, btk, _ = (ring or psS).next()
        for kc in range(nkc):
            sap, stk = src_fn(kc)
            mm(bank[0:M, 0:n], wt[:, kc, c0:c0 + M], sap, kc == 0, kc == nkc - 1, [wtk, stk] + CT, [btk], kc == nkc - 1)
        return bank, btk

    def h_src(tc):
        ts = slice(tc * TC, (tc + 1) * TC)
        return lambda kc: (hT[:, kc, ts], htok(kc, tc))

    def rope_evac(dst, dtk, A, Atk, B, Btk, p0, p1, ts, scale, tabs, ropetmp):
        cosT, sinT = tabs
        rt = ropetmp
        P.stt('dve', rt[p0:p1, :], A[p0:p1, :], scale, cosT[p0:p1, ts], ALU.mult, ALU.mult,
              [Atk, P.tok('tabs')], [P.tok('ropetmp')])
        P.stt('dve', B[p0:p1, :], B[p0:p1, :], scale, sinT[p0:p1, ts], ALU.mult, ALU.mult,
              [Btk, P.tok('tabs')], [Btk])
        P.tt('dve', dst, rt[p0:p1, :], B[p0:p1, :], ALU.add, [P.tok('ropetmp'), Btk], [dtk])

    MX_MIXED, MX_TAB, MX_PT, MX_Q, MX_K, MX_V = 0, 32768, 40960, 44032, 48128, 52224
    MX_LAT, MX_KR, MX_NT, MX_WSC, MX_RDEN, MX_ROPE = 56320, 68608, 72704, 84992, 86016, 88064

    def attn_core(Qt, Kt, Vt, kdim, nchunks, nj_fn, causal, den_aug, qtok_fn, ktok_fn, vtok_fn, out_fn, pt_off, rden_off, extra=()):
        PTs = [view(pt_off + 1024 * i, [TC], BF16) for i in range(3)]
        rden = view(rden_off, [TC], F32)
        st = {'k': 0}

        def emit_S(i, j):
            ts = slice(i * TC, (i + 1) * TC)
            bank, btk, _ = psS.next()
            diag = causal and j >= 4 * i
            c0 = 128 * (j - 4 * i) if diag else 0
            mm(bank[:, c0:TC], Kt[0:kdim, j * 128:(j + 1) * 128], Qt[0:kdim, i * TC + c0:(i + 1) * TC], True, not diag,
               [ktok_fn(j), qtok_fn(i)] + CT + list(extra), [btk], not diag)
            if diag:
                mm(bank[:, c0:c0 + 128], ident4[:, 0, :], cmask[:, 0, 0:128], False, True, CT, [btk], True)
            return bank, btk, c0

        for i in range(nchunks):
            nj = nj_fn(i)
            acc, atk, _ = psA.next()
            if not den_aug:
                den, dtk, _ = psA.next()
            nxt = emit_S(i, 0)
            for j in range(nj):
                bank, btk, c0 = nxt
                if j + 1 < nj:
                    nxt = emit_S(i, j + 1)
                k = st['k'] % 3
                st['k'] += 1
                P.act(PTs[k][:, c0:TC], bank[:, c0:TC], AF.Exp, [btk], [P.tok('PT', k)])
                mm(acc[:, c0:TC], Vt[:, j, :], PTs[k][:, c0:TC], j == 0, j == nj - 1, [vtok_fn(j), P.tok('PT', k)] + list(extra), [atk], j == nj - 1)
                if not den_aug:
                    mm(den[:, :], ones_bf[:, :], PTs[k][:, :], j == 0, j == nj - 1, [P.tok('PT', k)], [dtk], j == nj - 1)
            if den_aug:
                P.recip('dve', rden[0:64, :], acc[64:128, :], [atk], [P.tok('rden')])
            else:
                P.recip('dve', rden[:, :], den[:, :], [dtk], [P.tok('rden')])
            out_fn(i, acc, atk, rden, P.tok('rden'))

    def vtok_evac(Vt, src_fn, wt, wtk, c0, nkc, ntiles, vtokname):
        for j0 in range(0, ntiles, 4):
            bank, btk, _ = psS.next()
            for jj in range(4):
                j = j0 + jj
                for kc in range(nkc):
                    sap, stk = src_fn(kc, j)
                    mm(bank[:, jj * 64:(jj + 1) * 64], sap, wt[:, kc, c0:c0 + 64], kc == 0, kc == nkc - 1,
                       [wtk, stk] + CT, [btk], kc == nkc - 1 and jj == 3)
            P.copy('act' if (j0 // 4) % 2 else 'dve', Vt[:, j0:j0 + 4, 0:64],
                   bank[:, 0:256].rearrange("p (a b) -> p a b", a=4), [btk], [P.tok(vtokname, j0 // 4)])

    def mixed_out_fn(mixedT, chunk, pbase):
        def f(i, acc, atk, rden, rtk):
            ts = slice(i * TC, (i + 1) * TC)
            P.tt('dve', mixedT[pbase:pbase + 64, chunk, ts], acc[0:64, :], rden[0:64, :], ALU.mult,
                 [atk, rtk], [P.tok('mixed', chunk, i)])
        return f

    def mixer(l):
        mixedT = view(MX_MIXED, [8, T], BF16)
        tabs2 = view(MX_TAB, [2, T], BF16)
        tabs = (tabs2[:, 0, :], tabs2[:, 1, :])
        Qt = view(MX_Q, [T], BF16)
        Kt = view(MX_K, [T], BF16)
        Vt = view(MX_V, [16, 128], BF16)
        ropetmp = view(MX_ROPE, [TC], F32)
        win = fm(win_d[l])
        winx = fm(winx_d[l])
        norm_x(l * 5 + 1, MX_NT)
        P.barrier()
        if cfg.get('mix') is not None:
            for c in range(8):
                P.memset('dve', mixedT[:, c, :], 0.0, [], [P.tok('mixed', c, tc) for tc in range(NTC)])
        P.memset('dve', Vt[:, :, 64:128], 1.0, [], [P.tok('Vones')])
        CTm = [P.tok('Vones')]

        def qtok(i):
            return P.tok('Q', i)

        def ktok(j):
            return P.tok('K', j // 4)

        def vtok(j):
            return P.tok('V', j // 4)

        if 'fox' in (cfg.get('mix') or {'fox', 'mla', 'dsa'}):
            ones_f = view(MX_TAB, [T], F32)
            sp_f = view(MX_NT, [T], F32)
            cum_f = view(MX_Q, [T], F32)
            c3 = view(MX_LAT, [3, T], BF16)
            wt, wtk = wslot([(0, 8, 0, 6, win[:, :, O_FF:O_FF + 6])])
            P.memset('dve', ones_f[0:6, :], 1.0, [], [P.tok('ones_f')])
            for tc in range(NTC):
                ts = slice(tc * TC, (tc + 1) * TC)
                bank, btk = proj(wt, wtk, 0, 6, h_src(tc), 8, tc)
                P.act(sp_f[0:6, ts], bank[0:6, :], AF.Exp, [btk, P.tok('nbf')], [P.tok('sp')], bias=nbf[0:6, l:l + 1], scale=-1.0)
            P.act(sp_f[0:6, :], sp_f[0:6, :], AF.Ln, [P.tok('sp')], [P.tok('sp')], bias=1.0)
            P.op('dve', lambda e: e.tensor_tensor_scan(out=cum_f[0:6, :], data0=ones_f[0:6, :], data1=sp_f[0:6, :],
                                                        initial=0.0, op0=ALU.mult, op1=ALU.subtract),
                 [P.tok('sp'), P.tok('ones_f')], [P.tok('cum')])
            P.copy('dve', c3[0:6, 0, :], cum_f[0:6, :], [P.tok('cum')], [P.tok('c3')])
            P.tt('dve', sp_f[0:6, :], cum_f[0:6, :], c3[0:6, 0, :], ALU.subtract, [P.tok('cum'), P.tok('c3')], [P.tok('sp')])
            P.copy('dve', c3[0:6, 1, :], sp_f[0:6, :], [P.tok('sp')], [P.tok('c3')])
            P.tt('dve', cum_f[0:6, :], sp_f[0:6, :], c3[0:6, 1, :], ALU.subtract, [P.tok('sp'), P.tok('c3')], [P.tok('cum')])
            P.copy('dve', c3[0:6, 2, :], cum_f[0:6, :], [P.tok('cum')], [P.tok('c3')])
            P.barrier()
            for h in range(6):
                wt, wtk = wslot([(0, 8, 0, 64, win[:, :, O_FQ + h * 64:O_FQ + (h + 1) * 64]),
                                 (0, 8, 64, 128, win[:, :, O_FK + h * 64:O_FK + (h + 1) * 64]),
                                 (0, 8, 128, 192, win[:, :, O_FV + h * 64:O_FV + (h + 1) * 64])])
                P.memset('dve', Qt[64:70, :], -1.0, [], [P.tok('Qaug')])
                P.memset('dve', Kt[64:70, :], 1.0, [], [P.tok('Kaug')])
                for k3 in range(3):
                    P.dma('sp', 'augq', Qt[64 + k3:65 + k3, :], c3[h:h + 1, k3, :], reads=[P.tok('c3')], writes=[P.tok('Qaug')])
                    P.dma('sp', 'augk', Kt[67 + k3:68 + k3, :], c3[h:h + 1, k3, :], reads=[P.tok('c3')], writes=[P.tok('Kaug')])
                for tc in range(NTC):
                    ts = slice(tc * TC, (tc + 1) * TC)
                    bank, btk = proj(wt, wtk, 0, 64, h_src(tc), 8, tc)
                    P.act(Qt[0:64, ts], bank[0:64, :], AF.Copy, [btk], [P.tok('Q', tc)], scale=0.125)
                    bank, btk = proj(wt, wtk, 64, 64, h_src(tc), 8, tc)
                    P.copy('dve', Kt[0:64, ts], bank[0:64, :], [btk], [P.tok('K', tc)])
                vtok_evac(Vt, lambda kc, j: (hT[:, kc, j * 128:(j + 1) * 128], htok(kc, j // 4)), wt, wtk, 128, 8, 16, 'V')
                _attn_aug(Qt, Kt, Vt, 70, mixedT, h // 2, (h % 2) * 64, extra=[P.tok('Qaug'), P.tok('Kaug')] + CTm)
            P.barrier()

        if 'mla' in (cfg.get('mix') or {'fox', 'mla', 'dsa'}):
            P.dma('pool', 'tabs', tabs2[:, 0, :], tab_d[0], writes=[P.tok('tabs')])
            P.dma('pool', 'tabs', tabs2[:, 1, :], tab_d[1], writes=[P.tok('tabs')])
            cqn = view(MX_LAT, [2, T], BF16)
            ckvn = view(MX_LAT + 8192, [T], BF16)
            kr = view(MX_KR, [T], BF16)
            wtA, wtAk = wslot([(0, 8, 0, 256, win[:, :, O_CQ:O_CQ + 256])])
            wtB, wtBk = wslot([(0, 8, 0, 128, win[:, :, O_CKV:O_CKV + 128]),
                               (0, 8, 128, 160, win[:, :, O_KR:O_KR + 32]),
                               (0, 8, 160, 192, winx[:, :, X_KR:X_KR + 32])])
            for tc in range(NTC):
                ts = slice(tc * TC, (tc + 1) * TC)
                b0, b0k = proj(wtA, wtAk, 0, 128, h_src(tc), 8, tc)
                b1, b1k = proj(wtA, wtAk, 128, 128, h_src(tc), 8, tc)
                norm_chunk([(b0[:, :], b0k), (b1[:, :], b1k)], 256, lambda c: small[:, l * 2 + c:l * 2 + c + 1],
                           [(cqn[:, 0, ts], P.tok('cqn', tc)), (cqn[:, 1, ts], P.tok('cqn', tc))], TC, MX_NT)
                b2, b2k = proj(wtB, wtBk, 0, 128, h_src(tc), 8, tc)
                norm_chunk([(b2[:, :], b2k)], 128, lambda c: small[:, 4 + l:5 + l],
                           [(ckvn[:, ts], P.tok('ckvn', tc))], TC, MX_NT)
                A, Ak = proj(wtB, wtBk, 128, 32, h_src(tc), 8, tc)
                B, Bk = proj(wtB, wtBk, 160, 32, h_src(tc), 8, tc)
                rope_evac(kr[0:32, ts], P.tok('kr', tc), A, Ak, B, Bk, 0, 32, ts, 1.0, tabs, ropetmp)
            sc = 96.0 ** -0.5
            for h in range(6):
                wt, wtk = wslot([(0, 2, 0, 96, fm(wuq_d[l])[:, :, h * 96:(h + 1) * 96]),
                                 (0, 2, 96, 192, fm(wuqs_d[l])[:, :, h * 96:(h + 1) * 96]),
                                 (2, 3, 0, 128, fm(wukv_d[l])[:, :, h * 128:(h + 1) * 128])])
                for tc in range(NTC):
                    ts = slice(tc * TC, (tc + 1) * TC)
                    csrc = lambda kc, tc=tc, ts=ts: (cqn[:, kc, ts], P.tok('cqn', tc))
                    A, Ak = proj(wt, wtk, 0, 96, csrc, 2, tc)
                    B, Bk = proj(wt, wtk, 96, 96, csrc, 2, tc)
                    P.act(Qt[0:64, ts], A[0:64, :], AF.Copy, [Ak], [P.tok('Q', tc)], scale=sc)
                    rope_evac(Qt[64:96, ts], P.tok('Q', tc), A, Ak, B, Bk, 64, 96, ts, sc, tabs, ropetmp)
                    bank, btk = psS.next()[0:2]
                    mm(bank[0:64, :], wt[:, 2, 0:64], ckvn[:, ts], True, True, [wtk, P.tok('ckvn', tc)] + CT, [btk], True)
                    P.copy('dve', Kt[0:64, ts], bank[0:64, :], [btk], [P.tok('K', tc)])
                    P.copy('act', Kt[64:96, ts], kr[0:32, ts], [P.tok('kr', tc)], [P.tok('K', tc)])
                vtok_evac(Vt, lambda kc, j: (ckvn[:, j * 128:(j + 1) * 128], P.tok('ckvn', j // 4)), wt[:, 2:3, :], wtk,
                          64, 1, 16, 'V')
                _attn_aug(Qt, Kt, Vt, 96, mixedT, 3 + h // 2, (h % 2) * 64, extra=CTm)
            P.barrier()

        if 'dsa' in (cfg.get('mix') or {'fox', 'mla', 'dsa'}):
            dsa(l, mixedT, tabs2, tabs, ropetmp, win, winx, CTm)

        wo_v = fm(wout_d[l])
        for m in range(8):
            wt, wtk = wslot([(0, 8, 0, 128, wo_v[:, :, m * 128:(m + 1) * 128])])
            for tc in range(NTC):
                ts = slice(tc * TC, (tc + 1) * TC)
                ob, otk, _ = psA.next()
                for c in range(8):
                    mm(ob[:, :], wt[:, c, 0:128], mixedT[:, c, ts], c == 0, c == 7,
                       [wtk, P.tok('mixed', c, tc)] + CT, [otk], c == 7)
                P.tt('dve', xT[:, m, ts], ob[:, :], xT[:, m, ts], ALU.add, [otk, xtok(m, tc)], [xtok(m, tc)])
        P.barrier()

    def _attn_aug(Qt, Kt, Vt, kdim, mixedT, chunk, pbase, extra):
        attn_core(Qt, Kt, Vt, kdim, NTC, lambda i: 4 * (i + 1), True, True,
                  lambda i: P.tok('Q', i), lambda j: P.tok('K', j // 4), lambda j: P.tok('V', j // 4),
                  mixed_out_fn(mixedT, chunk, pbase), MX_PT, MX_RDEN, extra=extra)

    def dsa(l, mixedT, tabs2, tabs, ropetmp, win, winx, CTm):
        NIT = 14
        BR = 8.0
        qi = view(MX_LAT, [3, T], BF16)
        ki = view(MX_KR, [T], BF16)
        qc = view(MX_Q, [2, T], BF16)
        kc_t = view(MX_NT, [T], BF16)
        Vt = view(MX_V, [16, 128], BF16)
        wsc = view(MX_WSC, [2, 128], F32)
        P.dma('pool', 'tabs', tabs2[:, 0, :], tab_d[0], writes=[P.tok('tabs')])
        P.dma('pool', 'tabs', tabs2[:, 1, :], tab_d[1], writes=[P.tok('tabs')])
        for g in range(3):
            M = 96 if g < 2 else 64
            wt, wtk = wslot([(0, 8, 0, M, win[:, :, O_DQI + g * 96:O_DQI + g * 96 + M]),
                             (0, 8, 96, 96 + M, winx[:, :, X_DQI + g * 96:X_DQI + g * 96 + M])])
            for tc in range(NTC):
                ts = slice(tc * TC, (tc + 1) * TC)
                A, Ak = proj(wt, wtk, 0, M, h_src(tc), 8, tc)
                B, Bk = proj(wt, wtk, 96, M, h_src(tc), 8, tc)
                rope_evac(qi[0:M, g, ts], P.tok('qi', g, tc), A, Ak, B, Bk, 0, M, ts, 1.0, tabs, ropetmp)
        wt, wtk = wslot([(0, 8, 0, 96, winx[:, :, X_KI:X_KI + 96]), (0, 8, 96, 192, winx[:, :, X_KIS:X_KIS + 96]),
                         (0, 8, 192, 200, win[:, :, O_DWI:O_DWI + 8])])
        for tc in range(NTC):
            ts = slice(tc * TC, (tc + 1) * TC)
            A, Ak = proj(wt, wtk, 0, 96, h_src(tc), 8, tc)
            B, Bk = proj(wt, wtk, 96, 96, h_src(tc), 8, tc)
            rope_evac(ki[0:96, ts], P.tok('ki', tc), A, Ak, B, Bk, 0, 96, ts, 1.0, tabs, ropetmp)
        bank, btk, _ = psS.next()
        for j in range(16):
            for kc in range(8):
                mm(bank[:, j * 8:(j + 1) * 8], hT[:, kc, j * 128:(j + 1) * 128], wt[:, kc, 192:200], kc == 0, kc == 7,
                   [wtk, htok(kc, j // 4)] + CT, [btk], kc == 7 and j == 15)
        P.copy('act', wsc[:, 0, :], bank[:, 0:128], [btk], [P.tok('wsc')])
        P.barrier()
        P.dma('pool', 'tabs', tabs2[:, 0, :], tab_d[2], writes=[P.tok('tabs')])
        P.dma('pool', 'tabs', tabs2[:, 1, :], tab_d[3], writes=[P.tok('tabs')])
        for sl in range(2):
            wt, wtk = wslot([(0, 8, 0, 128, win[:, :, O_DQ + sl * 128:O_DQ + (sl + 1) * 128]),
                             (0, 8, 128, 256, winx[:, :, X_DQ + sl * 128:X_DQ + (sl + 1) * 128])])
            for tc in range(NTC):
                ts = slice(tc * TC, (tc + 1) * TC)
                A, Ak = proj(wt, wtk, 0, 128, h_src(tc), 8, tc)
                B, Bk = proj(wt, wtk, 128, 128, h_src(tc), 8, tc)
                rope_evac(qc[:, sl, ts], P.tok('qc', tc), A, Ak, B, Bk, 0, 128, ts, 0.125, tabs, ropetmp)
        wt, wtk = wslot([(0, 8, 0, 64, win[:, :, O_DK:O_DK + 64]), (0, 8, 64, 128, win[:, :, O_DK:O_DK + 64]),
                         (0, 8, 128, 192, winx[:, :, X_DK:X_DK + 64]), (0, 8, 192, 256, winx[:, :, X_DK:X_DK + 64])])
        for tc in range(NTC):
            ts = slice(tc * TC, (tc + 1) * TC)
            A, Ak = proj(wt, wtk, 0, 128, h_src(tc), 8, tc)
            B, Bk = proj(wt, wtk, 128, 128, h_src(tc), 8, tc)
            rope_evac(kc_t[:, ts], P.tok('kc', tc), A, Ak, B, Bk, 0, 128, ts, 1.0, tabs, ropetmp)
        wt, wtk = wslot([(0, 8, 0, 64, win[:, :, O_DV:O_DV + 64])])
        vtok_evac(Vt, lambda kc, j: (hT[:, kc, j * 128:(j + 1) * 128], htok(kc, j // 4)), wt, wtk, 0, 8, 16, 'V')
        P.barrier()
        hflat = hT[:, 0:8, :].rearrange("p a b -> p (a b)")
        iscs = [hflat[:, 4096 * k:4096 * (k + 1)].bitcast(F32) for k in range(2)]
        negms = [hflat[:, 8192 + 2048 * k: 8192 + 2048 * (k + 1)] for k in range(2)]
        rbufs = [hflat[:, 12288 + 512 * k: 12288 + 512 * (k + 1)] for k in range(4)]
        Dts = [hflat[:, 14336 + 1024 * k: 14336 + 1024 * (k + 1)].rearrange("p (a b) -> p a b", a=8) for k in range(2)]
        bis = _cache.get(('bis', l))
        if bis is None:
            bis = _cache[('bis', l)] = sb('bis%d' % l, [128, 8], F32)
        cnt, stp, lo = bis[:, 1:2], bis[:, 2:3], bis[:, 3:4]
        mids = [bis[:, 0:1], bis[:, 4:5]]
        PTs = [view(MX_PT + 1024 * i, [TC], BF16) for i in range(3)]
        rden = view(MX_RDEN, [TC], F32)
        ident_bf = ident4[:, 0, :]
        cts_bf = view(MX_ROPE, [128], BF16)
        P.copy('dve', cts_bf[:, :], cts[:, :], CT, [P.tok('cts_bf')])
        RSC = (8.0 ** -0.5) * (32.0 ** -0.5)
        stc = {'rk': 0, 'kk': 0}

        def stageA(i):
            W = (i + 1) * 128
            tt_ = slice(i * 128, (i + 1) * 128)
            isc = iscs[i % 2]
            Dt = Dts[i % 2]
            for hh in range(8):
                P.ts('dve', Dt[:, hh, :], ident_bf, wsc[:, 0, i * 8 + hh:i * 8 + hh + 1], None, ALU.mult, None,
                     [P.tok('wsc')] + CT, [P.tok('Dt', i % 2)])
            for c0 in range(0, W, TC):
                n = min(TC, W - c0)
                ib, ibk, _ = psA.next()
                diag = (c0 + n == W)
                pend = []

                def flush_one():
                    hh2, rb2, rbk2 = pend.pop(0)
                    mm(ib[:, 0:n], Dt[:, hh2, :], rb2[:, 0:n], hh2 == 0, (hh2 == 7 and not diag), [rbk2, P.tok('Dt', i % 2)], [ibk],
                       hh2 == 7 and not diag)
                for hh in range(8):
                    g, r = hh // 3, hh % 3
                    bank, btk, _ = psS.next()
                    mm(bank[:, 0:n], qi[r * 32:(r + 1) * 32, g, tt_], ki[r * 32:(r + 1) * 32, c0:c0 + n], True, True,
                       [P.tok('qi', g, i // 4)] + [P.tok('ki', q) for q in range(c0 // TC, (c0 + n - 1) // TC + 1)] + CT,
                       [btk], True)
                    rb = rbufs[stc['rk'] % 4]
                    rbk = P.tok('rbuf', stc['rk'] % 4)
                    stc['rk'] += 1
                    P.act(rb[:, 0:n], bank[:, 0:n], AF.Relu, [btk], [rbk], scale=RSC)
                    pend.append((hh, rb, rbk))
                    if len(pend) > 2:
                        flush_one()
                while pend:
                    flush_one()
                if diag:
                    mm(ib[:, n - 128:n], ident_bf, cts_bf[:, :], False, True, [P.tok('cts_bf')], [ibk], True)
                P.copy('act', isc[:, c0:c0 + n], ib[:, 0:n], [ibk], [P.tok('isc', i % 2, c0)])

        def stageB(i):
            W = (i + 1) * 128
            tt_ = slice(i * 128, (i + 1) * 128)
            negm = negms[i % 2]
            ntk = P.tok('negm', i % 2)
            if i < 2:
                if i == 1:
                    P.memset('dve', negm[:, 0:128], 0.0, [], [ntk])
                P.ts('dve', negm[:, tt_], cts[:, :], NEG, None, ALU.max, None, CT, [ntk])
                return
            isc = iscs[i % 2]
            itoks = [P.tok('isc', i % 2, c0) for c0 in range(0, W, TC)]
            mid = mids[0]
            P.memset('dve', mid, 0.0, [], [P.tok('mid')])
            for it in range(NIT):
                d = BR / (2.0 ** it)
                P.ts('dve', negm[:, 0:W], isc[:, 0:W], mid, 0.0, ALU.is_ge, ALU.add, itoks + [P.tok('mid')],
                     [ntk, P.tok('cnt')], accum_out=cnt)
                P.ts('dve', stp, cnt, 255.5, 0.5, ALU.is_ge, ALU.subtract, [P.tok('cnt')], [P.tok('stp')])
                P.stt('dve', mid, stp, d, mid, ALU.mult, ALU.add, [P.tok('stp'), P.tok('mid')], [P.tok('mid')])
            P.ts('dve', lo, mid, -BR / (2.0 ** NIT), None, ALU.add, None, [P.tok('mid')], [P.tok('lo')])
            P.ts('dve', negm[:, 0:W], isc[:, 0:W], lo, NEG, ALU.is_lt, ALU.mult, itoks + [P.tok('lo')], [ntk])

        def stageC(i):
            tt_ = slice(i * 128, (i + 1) * 128)
            negm = negms[i % 2]
            ntk = P.tok('negm', i % 2)
            acc, atk, _ = psA.next()

            def emit_S(j):
                bank, btk, _ = psS.next()
                for p in range(2):
                    mm(bank[:, p * 256:(p + 1) * 256], kc_t[p * 64:(p + 1) * 64, j * 128:(j + 1) * 128],
                       qc[p * 64:(p + 1) * 64, :, tt_], True, False,
                       [P.tok('kc', j // 4), P.tok('qc', i // 4)] + CT, [btk], False)
                    mm(bank[:, p * 256:(p + 1) * 256], negm[:, j * 128:(j + 1) * 128], ident4[:, 0:2, :], False, True,
                       [ntk], [btk], p == 1)
                return bank, btk
            nxt = emit_S(0)
            for j in range(i + 1):
                bank, btk = nxt
                if j < i:
                    nxt = emit_S(j + 1)
                k = stc['kk'] % 3
                stc['kk'] += 1
                P.act(PTs[k][:, :], bank[:, :], AF.Exp, [btk], [P.tok('PT', k)])
                mm(acc[:, :], Vt[:, j, :], PTs[k][:, :], j == 0, j == i, [P.tok('V', j // 4), P.tok('PT', k)] + CTm,
                   [atk], j == i)
            P.recip('dve', rden[0:64, :], acc[64:128, :], [atk], [P.tok('rden')])
            for cb in range(4):
                p, sl = cb // 2, cb % 2
                P.tt('dve', mixedT[p * 64:(p + 1) * 64, 6 + sl, tt_], acc[0:64, cb * 128:(cb + 1) * 128],
                     rden[0:64, cb * 128:(cb + 1) * 128], ALU.mult, [atk, P.tok('rden')], [P.tok('mixed', 6 + sl, i // 4)])

        stageA(2)
        for i in range(16):
            stageB(i)
            if 2 <= i + 1 <= 15 and i + 1 != 2:
                stageA(i + 1)
            stageC(i)
        P.barrier()

    _cache = {}

    def xattn(l, s):
        oxT = view(0, [4, T], BF16)
        memT = view(16384, [8, MEM], F32)
        hm = view(24576, [8, MEM], BF16)
        XNT = 28672
        Qt = view(MX_Q, [T], BF16)
        Kt = view(MX_K, [MEM], BF16)
        Vt = view(MX_V, [2, 128], BF16)
        norm_x(l * 5 + 2, XNT)
        for c in range(8):
            P.dma('sp', 'memld%d' % c, memT[:, c, :], memT_d[s, c * 128:(c + 1) * 128, :], writes=[P.tok('memT', c)])
        norm_chunk([(memT[:, c, :], P.tok('memT', c)) for c in range(8)], D, lambda c: gain_ap(l * 5 + 3, c),
                   [(hm[:, c, :], P.tok('hm', c)) for c in range(8)], MEM, XNT)
        sc = 128.0 ** -0.5
        for h in range(4):
            wq, wqk = wslot([(0, 8, 0, 128, fm(xwq_d[l])[:, :, h * 128:(h + 1) * 128])])
            wkv, wkvk = wslot([(0, 8, 0, 128, fm(xwkv_d[l])[:, :, h * 128:(h + 1) * 128]),
                               (0, 8, 128, 256, fm(xwkv_d[l])[:, :, 512 + h * 128:512 + (h + 1) * 128])])
            for tc in range(NTC):
                ts = slice(tc * TC, (tc + 1) * TC)
                bank, btk = proj(wq, wqk, 0, 128, h_src(tc), 8, tc)
                P.act(Qt[:, ts], bank[:, :], AF.Copy, [btk], [P.tok('Q', tc)], scale=sc)
            bank, btk = proj(wkv, wkvk, 0, 128, lambda kc: (hm[:, kc, :], P.tok('hm', kc)), 8, 0, n=MEM)
            P.copy('dve', Kt[:, :], bank[:, 0:MEM], [btk], [P.tok('K', 0)])
            bank, btk, _ = psS.next()
            for j in range(2):
                for kc in range(8):
                    mm(bank[:, j * 128:(j + 1) * 128], hm[:, kc, j * 128:(j + 1) * 128], wkv[:, kc, 128:256], kc == 0, kc == 7,
                       [wkvk, P.tok('hm', kc)] + CT, [btk], kc == 7 and j == 1)
            P.copy('dve', Vt[:, :, :], bank[:, 0:256].rearrange("p (a b) -> p a b", a=2), [btk], [P.tok('V', 0)])

            def out_fn(i, acc, atk, rden, rtk, h=h):
                ts = slice(i * TC, (i + 1) * TC)
                P.tt('dve', oxT[:, h, ts], acc[:, :], rden[:, :], ALU.mult, [atk, rtk], [P.tok('ox', h, i)])
            attn_core(Qt, Kt, Vt, 128, NTC, lambda i: 2, False, False, lambda i: P.tok('Q', i),
                      lambda j: P.tok('K', 0), lambda j: P.tok('V', 0), out_fn, MX_PT, MX_RDEN)
        xo = xwo_d[l].rearrange("(hc p) n -> p hc n", p=128)
        for m in range(8):
            wt, wtk = wslot([(0, 4, 0, 128, xo[:, :, m * 128:(m + 1) * 128])])
            for tc in range(NTC):
                ts = slice(tc * TC, (tc + 1) * TC)
                ob, otk, _ = psA.next()
                for h in range(4):
                    mm(ob[:, :], wt[:, h, 0:128], oxT[:, h, ts], h == 0, h == 3, [wtk, P.tok('ox', h, tc)] + CT, [otk], h == 3)
                P.tt('dve', xT[:, m, ts], ob[:, :], xT[:, m, ts], ALU.add, [otk, xtok(m, tc)], [xtok(m, tc)])
        P.barrier()

    def final_norm(s):
        NT_OFF = 0
        ob = [view(16384 + 2048 * i, [TC], F32) for i in range(4)]
        k = [0]

        def dst_fn(c, t0, n):
            return ob[k[0] % 4][:, 0:n]

        sq = view(NT_OFF, [8, TC], BF16)
        s_t = view(NT_OFF + 8192, [TC], F32)
        r_t = view(NT_OFF + 10240, [TC], F32)
        for tc in range(NTC):
            t0 = tc * TC
            for c in range(8):
                P.act(sq[:, c, :], xT[:, c, t0:t0 + TC], AF.Square, [xtok(c, tc)], [P.tok('nt_sq', c)])
            bank, btk, _ = psM.next()
            for c in range(8):
                mm(bank[:, :], ones_bf[:, :], sq[:, c, :], c == 0, c == 7, [P.tok('nt_sq', c)] + CT, [btk], c == 7)
            P.act(s_t[:, :], bank[:, :], AF.Sqrt, [btk], [P.tok('nt_s')], bias=eps_t[:, 0:1], scale=1.0 / D)
            P.recip('dve', r_t[:, :], s_t[:, :], [P.tok('nt_s')], [P.tok('nt_r')])
            for c in range(8):
                o = ob[k[0] % 4]
                otk = P.tok('fo', k[0] % 4)
                ost = 'outst%d' % (k[0] % 4)
                k[0] += 1
                P.stt('dve', o[:, :], xT[:, c, t0:t0 + TC], gain_ap(10, c), r_t[:, :], ALU.mult, ALU.mult,
                      [xtok(c, tc), P.tok('nt_r')], [otk])
                P.dma('sp', ost, out_d[s, c * 128:(c + 1) * 128, t0:t0 + TC], o[:, :], reads=[otk])
        P.barrier()

    for s in range(nseq):
        for c in range(8):
            P.dma('sp', 'xload%d' % c, xT[:, c, :], xT_d[s, c * 128:(c + 1) * 128, :],
                  writes=[xtok(c, tc) for tc in range(NTC)])
        for l in range(layers):
            if 'ffn1' in phases:
                ffn(l, 0, l * 5 + 0)
            if 'mix' in phases:
                mixer(l)
            if 'xa' in phases:
                xattn(l, s)
            if 'ffn2' in phases:
                ffn(l, 1, l * 5 + 4)
        if 'final' in phases:
            final_norm(s)
        else:
            for c in range(8):
                P.dma('sp', 'outst%d' % (c % 4), out_d[s, c * 128:(c + 1) * 128, :], xT[:, c, :],
                      reads=[xtok(c, tc) for tc in range(NTC)])
    for st_name, v in P.dcnt.items():
        if st_name.startswith('outst') and v > 0:
            P.need('sp', (st_name, v), True)
    P.emit()
    return es, P


def _fm(g):
    return np.ascontiguousarray(g.reshape(8, 128).T)


def host_prep(inputs, nseq_total=None):
    f = {k: np.asarray(v, dtype=np.float32) for k, v in inputs.items()}
    shared = {}
    for k in ['ffn1_wi', 'ffn2_wi', 'ffn1_wo', 'ffn2_wo', 'w_in', 'mla_w_uq', 'mla_w_ukv', 'w_out',
              'xa_wq', 'xa_wkv', 'xa_wo']:
        shared[k] = np.ascontiguousarray(f[k])
    w_in = f['w_in']
    def swap_idx(base, nheads, d):
        h = d // 2
        idx = []
        for hh in range(nheads):
            b = base + hh * d
            idx += list(range(b + h, b + d)) + list(range(b, b + h))
        return idx
    cols = (swap_idx(O_KR, 1, 32) + swap_idx(O_DQ, 4, 64) + swap_idx(O_DK, 1, 64) + swap_idx(O_DQI, 8, 32)
            + list(range(O_DKI, O_DKI + 32)) * 3 + swap_idx(O_DKI, 1, 32) * 3)
    assert len(cols) == NX
    shared['w_inx'] = np.ascontiguousarray(w_in[:, :, cols])
    uq_cols = []
    for hh in range(6):
        b = hh * 96
        uq_cols += list(range(b, b + 64)) + list(range(b + 80, b + 96)) + list(range(b + 64, b + 80))
    shared['mla_w_uqs'] = np.ascontiguousarray(f['mla_w_uq'][:, :, uq_cols])
    gains = np.zeros((128, 96), np.float32)
    for l in range(L):
        for i, k in enumerate(['ffn1_norm', 'mix_norm', 'xa_norm', 'mem_norm', 'ffn2_norm']):
            gains[:, (l * 5 + i) * 8:(l * 5 + i + 1) * 8] = _fm(f[k][l])
    gains[:, 80:88] = _fm(f['final_norm'])
    shared['gains'] = gains
    small = np.zeros((128, 16), np.float32)
    for l in range(L):
        small[:, l * 2:(l + 1) * 2] = f['mla_q_norm'][l].reshape(2, 128).T
        small[:, 4 + l] = f['mla_kv_norm'][l]
        small[0:6, 8 + l] = f['b_forget'][l]
    shared['small'] = small
    pos = np.arange(T, dtype=np.float32)
    p = np.arange(128)
    tabs = np.zeros((4, 128, T), np.float32)
    for ti, half in enumerate([16, 32]):
        inv = (10000.0 ** (-(np.arange(half, dtype=np.float32)) / half)).astype(np.float32)
        ang = pos[None, :] * inv[p % half][:, None]
        sign = np.where((p % (2 * half)) < half, -1.0, 1.0).astype(np.float32)
        tabs[2 * ti] = np.cos(ang)
        tabs[2 * ti + 1] = np.sin(ang) * sign[:, None]
    shared['tables'] = tabs
    sl = np.arange(128)[:, None]
    tl = np.arange(TC)[None, :]
    cm = np.zeros((128, 4, TC), np.float32)
    for r in range(4):
        cm[:, r, :] = np.where(tl >= sl + 128 * r, 0.0, NEG)
    shared['cmask'] = cm.reshape(128, 4 * TC)
    shared['ident4'] = np.tile(np.eye(128, dtype=np.float32), (1, 4))
    shared['causal_ts'] = np.where(np.arange(128)[None, :] <= np.arange(128)[:, None], 0.0, BIGNEG).astype(np.float32)
    xT = np.ascontiguousarray(np.transpose(f['x'], (0, 2, 1)))
    memT = np.ascontiguousarray(np.transpose(f['mem'], (0, 2, 1)))
    return shared, xT, memT


def kernel(**inputs):
    shared, xT, memT = host_prep(inputs)
    ncore = 8
    nc = bass.Bass("TRN2", target_bir_lowering=False)
    es, P = build(nc, {})
    in_maps = []
    for c in range(ncore):
        m = dict(shared)
        m['xT'] = xT[c * NSEQ:(c + 1) * NSEQ]
        m['memT'] = memT[c * NSEQ:(c + 1) * NSEQ]
        in_maps.append(m)
    res = run_bass_kernel_spmd(nc, in_maps, core_ids=list(range(ncore)))
    es.close()
    outT = np.concatenate([r['outT'] for r in res.results], axis=0)
    return np.ascontiguousarray(np.transpose(outT, (0, 2, 1))).astype(np.float32)
```

```python
import numpy as np
from contextlib import ExitStack
import concourse.bass as bass
import concourse.mybir as mybir
from concourse.bass_utils import run_bass_kernel_spmd

F32 = mybir.dt.float32
BF16 = mybir.dt.bfloat16
AF = mybir.ActivationFunctionType
ALU = mybir.AluOpType

D = 1024
T = 2048
L = 2
NSEQ = 2
DFF = 2816
NF = 22
MEM = 256
TC = 512
NTC = T // TC
EPS = 1e-6
NEG = -30000.0
BIGNEG = -1.0e30
ENG = ['pe', 'act', 'dve', 'pool', 'sp']

O_FQ, O_FK, O_FV, O_FF = 0, 384, 768, 1152
O_CQ, O_CKV, O_KR = 1158, 1414, 1542
O_DQ, O_DK, O_DV, O_DQI, O_DKI, O_DWI = 1574, 1830, 1894, 1958, 2214, 2246
X_KR, X_DQ, X_DK, X_DQI, X_KI, X_KIS = 0, 32, 288, 352, 608, 704
NX = 800


class Tok:
    __slots__ = ('w', 'r')

    def __init__(self):
        self.w = None
        self.r = {}


class Prog:
    def __init__(self, nc, es):
        self.nc = nc
        self.es = es
        self.semh = {e: es.enter_context(nc.semaphore('s_' + e)) for e in ENG}
        self.cnt = {e: 0 for e in ENG}
        self.seen = {e: {} for e in ENG}
        self.q = {e: [] for e in ENG}
        self.clock = {e: [None] for e in ENG}
        self.toks = {}
        self.dcnt = {}
        self.nwait = 0

    def tok(self, *key):
        t = self.toks.get(key)
        if t is None:
            t = self.toks[key] = Tok()
        return t

    def stream(self, name):
        if name not in self.semh:
            self.semh[name] = self.es.enter_context(self.nc.semaphore('d_' + name))
            self.dcnt[name] = 0
        return name

    def need(self, eng, ev, raw):
        if ev is None:
            return
        key, val = ev
        if key == eng and (eng == 'pe' or eng == 'sp' or not raw):
            return
        sn = self.seen[eng]
        if sn.get(key, 0) >= val:
            return
        sem = self.semh[key]
        self.q[eng].append(lambda e, sem=sem, val=val: e.wait_ge(sem, val))
        self.nwait += 1
        sn[key] = val
        if key in self.clock and val < len(self.clock[key]):
            for k2, v2 in self.clock[key][val].items():
                if sn.get(k2, 0) < v2:
                    sn[k2] = v2

    def _deps(self, eng, reads, writes):
        for t in reads:
            self.need(eng, t.w, True)
        for t in writes:
            self.need(eng, t.w, False)
            for k, v in t.r.items():
                self.need(eng, (k, v), False)

    def _mark(self, ev, reads, writes):
        k, v = ev
        for t in reads:
            if t.r.get(k, 0) < v:
                t.r[k] = v
        for t in writes:
            t.w = ev
            t.r = {}

    def op(self, eng, fn, reads=(), writes=(), inc=True):
        self._deps(eng, reads, writes)
        if inc:
            self.cnt[eng] += 1
            ev = (eng, self.cnt[eng])
            sem = self.semh[eng]
            self.q[eng].append(lambda e, fn=fn, sem=sem: fn(e).then_inc(sem, 1))
            self.clock[eng].append(dict(self.seen[eng]))
        else:
            ev = (eng, self.cnt[eng] + 1)
            self.q[eng].append(lambda e, fn=fn: fn(e))
        self._mark(ev, reads, writes)

    def dma(self, eng, stream, out, in_, reads=(), writes=()):
        self.stream(stream)
        self._deps(eng, reads, writes)
        self.dcnt[stream] += 16
        ev = (stream, self.dcnt[stream])
        sem = self.semh[stream]
        self.q[eng].append(lambda e, out=out, in_=in_, sem=sem: e.dma_start(out=out, in_=in_).then_inc(sem, 16))
        self._mark(ev, reads, writes)
        return ev

    def act(self, out, in_, func, reads, writes, bias=None, scale=None):
        kw = {}
        if bias is not None:
            kw['bias'] = bias
        if scale is not None:
            kw['scale'] = scale
        self.op('act', lambda e: e.activation(out=out, in_=in_, func=func, **kw), reads, writes)

    def stt(self, eng, out, in0, scalar, in1, op0, op1, reads, writes):
        self.op(eng, lambda e: e.scalar_tensor_tensor(out=out, in0=in0, scalar=scalar, in1=in1, op0=op0, op1=op1),
                reads, writes)

    def tt(self, eng, out, in0, in1, op, reads, writes):
        self.op(eng, lambda e: e.tensor_tensor(out=out, in0=in0, in1=in1, op=op), reads, writes)

    def ts(self, eng, out, in0, s1, s2, op0, op1, reads, writes, accum_out=None):
        kw = {}
        if accum_out is not None:
            kw['accum_out'] = accum_out
        if op1 is None:
            self.op(eng, lambda e: e.tensor_scalar(out=out, in0=in0, scalar1=s1, scalar2=None, op0=op0, **kw),
                    reads, writes)
        else:
            self.op(eng, lambda e: e.tensor_scalar(out=out, in0=in0, scalar1=s1, scalar2=s2, op0=op0, op1=op1, **kw),
                    reads, writes)

    def recip(self, eng, out, in_, reads, writes):
        self.op(eng, lambda e: e.reciprocal(out=out, in_=in_), reads, writes)

    def copy(self, eng, out, in_, reads, writes):
        if eng == 'act':
            self.op(eng, lambda e: e.copy(out=out, in_=in_), reads, writes)
        else:
            self.op(eng, lambda e: e.tensor_copy(out=out, in_=in_), reads, writes)

    def memset(self, eng, out, val, reads, writes):
        self.op(eng, lambda e: e.memset(out, val), reads, writes)

    def barrier(self):
        for e in ['pe', 'act', 'dve', 'pool', 'sp']:
            for o in ['pe', 'act', 'dve', 'pool']:
                if o != e and self.cnt[o] > 0:
                    self.need(e, (o, self.cnt[o]), True)
            for st, v in self.dcnt.items():
                if v > 0 and not (st.startswith('wi') or st.startswith('wo')):
                    self.need(e, (st, v), True)

    def emit(self):
        nc = self.nc
        q = self.q
        with nc.Block() as block:
            @block.tensor
            def _(e):
                for f in q['pe']:
                    f(e)

            @block.scalar
            def _(e):
                for f in q['act']:
                    f(e)

            @block.vector
            def _(e):
                for f in q['dve']:
                    f(e)

            @block.gpsimd
            def _(e):
                for f in q['pool']:
                    f(e)

            @block.sync
            def _(e):
                for f in q['sp']:
                    f(e)


class Ring:
    def __init__(self, P, name, tiles):
        self.P = P
        self.name = name
        self.tiles = tiles
        self.i = 0

    def next(self):
        k = self.i % len(self.tiles)
        self.i += 1
        return self.tiles[k], self.P.tok(self.name, k), '%s%d' % (self.name, k)


def build(nc, cfg):
    nseq = cfg.get('nseq', NSEQ)
    layers = cfg.get('layers', L)
    phases = cfg.get('phases', {'ffn1', 'mix', 'xa', 'ffn2', 'final'})
    es = ExitStack()
    P = Prog(nc, es)

    used_inputs = cfg.get('inputs', None)

    def din(name, shape, dt=F32):
        if used_inputs is not None and name not in used_inputs:
            return None
        return nc.dram_tensor(name, list(shape), dt, kind="ExternalInput").ap()

    xT_d = din('xT', [nseq, D, T])
    memT_d = din('memT', [nseq, D, MEM])
    wi_d = [din('ffn1_wi', [L, D, 2 * DFF]), din('ffn2_wi', [L, D, 2 * DFF])]
    wo_d = [din('ffn1_wo', [L, DFF, D]), din('ffn2_wo', [L, DFF, D])]
    win_d = din('w_in', [L, D, 2254])
    winx_d = din('w_inx', [L, D, NX])
    wuq_d = din('mla_w_uq', [L, 256, 576])
    wuqs_d = din('mla_w_uqs', [L, 256, 576])
    wukv_d = din('mla_w_ukv', [L, 128, 768])
    wout_d = din('w_out', [L, D, D])
    xwq_d = din('xa_wq', [L, D, 512])
    xwkv_d = din('xa_wkv', [L, D, 1024])
    xwo_d = din('xa_wo', [L, 512, D])
    gains_d = din('gains', [128, 96])
    small_d = din('small', [128, 16])
    tab_d = din('tables', [4, 128, T])
    cmask_d = din('cmask', [128, 4 * TC])
    ident_d = din('ident4', [128, 4 * 128])
    cts_d = din('causal_ts', [128, 128])
    out_d = nc.dram_tensor('outT', [nseq, D, T], F32, kind="ExternalOutput").ap()

    def sb(name, shape, dt):
        return es.enter_context(nc.sbuf_tensor('sb_' + name, list(shape), dt))

    def ps(name):
        return es.enter_context(nc.psum_tensor(name, [128, TC], F32))

    xT = sb('xT', [128, 8, T], F32)
    hT = sb('hT', [128, 8, T], BF16)
    ident4 = sb('ident4', [128, 4, 128], BF16)
    cmask = sb('cmask', [128, 4, TC], BF16)
    ones_bf = sb('ones_bf', [128, 128], BF16)
    cts = sb('cts', [128, 128], F32)
    gains = sb('gains', [128, 96], F32)
    small = sb('small', [128, 16], F32)
    nbf = sb('nbf', [128, 2], F32)
    wi_tiles = [sb('wiring%d' % i, [128, 8, 256], BF16) for i in range(3)]
    wo_tiles = [sb('woring%d' % i, [128, 11, 128], BF16) for i in range(2)]
    wi_ring = Ring(P, 'wi', wi_tiles)
    wo_ring = Ring(P, 'wo', wo_tiles)
    SCR_BYTES = 90112
    scr = sb('scr', [128, SCR_BYTES // 4], F32)

    def view(off, shape, dt):
        n = int(np.prod(shape))
        esz = 4 if dt == F32 else 2
        assert off % 4 == 0 and (n * esz) % 4 == 0 and off + n * esz <= SCR_BYTES, (off, shape)
        a = scr[:, off // 4: off // 4 + (n * esz) // 4]
        if dt != F32:
            a = a.bitcast(dt)
        if len(shape) == 2:
            a = a.rearrange("p (a b) -> p a b", a=shape[0])
        elif len(shape) == 3:
            a = a.rearrange("p (a b c) -> p a b c", a=shape[0], b=shape[1])
        return a

    banks = [ps('bank%d' % i) for i in range(8)]
    psS = Ring(P, 'psS', banks[0:4])
    psA = Ring(P, 'psA', banks[4:7])
    psM = Ring(P, 'psM', banks[7:8])

    def mm(out, lhsT, rhs, start, stop, reads, writes, inc):
        P.op('pe', lambda e: e.matmul(out, lhsT, rhs, start=start, stop=stop), reads=reads, writes=writes, inc=inc)

    def load_w(ring, dram_ap, ncols_total=None, parts=None):
        tile, tk, st = ring.next()
        for i, (dst_fn, src) in enumerate(parts):
            P.dma('pool', st, dst_fn(tile), src, writes=[tk])
        return tile, tk

    def gain_ap(idx, c):
        return gains[:, idx * 8 + c: idx * 8 + c + 1]

    def norm_chunk(srcs, nfeat, gain_fn, dsts, n, nt_off):
        sq = view(nt_off, [8, TC], BF16)
        s_t = view(nt_off + 8192, [TC], F32)
        r_t = view(nt_off + 10240, [TC], F32)
        nch = len(srcs)
        for c, (sap, stk) in enumerate(srcs):
            P.act(sq[:, c, 0:n], sap, AF.Square, [stk], [P.tok('nt_sq', c)])
        bank, btk, _ = psM.next()
        for c in range(nch):
            mm(bank[:, 0:n], ones_bf[:, :], sq[:, c, 0:n], c == 0, c == nch - 1,
               [P.tok('nt_sq', c)] + CT, [btk], c == nch - 1)
        P.act(s_t[:, 0:n], bank[:, 0:n], AF.Sqrt, [btk], [P.tok('nt_s')], bias=eps_t[:, 0:1], scale=1.0 / nfeat)
        P.recip('dve', r_t[:, 0:n], s_t[:, 0:n], [P.tok('nt_s')], [P.tok('nt_r')])
        for c, ((sap, stk), (dap, dtk)) in enumerate(zip(srcs, dsts)):
            P.stt('dve', dap, sap, gain_fn(c), r_t[:, 0:n], ALU.mult, ALU.mult, [stk, P.tok('nt_r')], [dtk])

    def norm_x(gidx, nt_off):
        for tc in range(NTC):
            ts = slice(tc * TC, (tc + 1) * TC)
            norm_chunk([(xT[:, c, ts], xtok(c, tc)) for c in range(8)], D, lambda c: gain_ap(gidx, c),
                       [(hT[:, c, ts], htok(c, tc)) for c in range(8)], TC, nt_off)

    eps_t = sb('eps_t', [128, 1], F32)

    P.dma('sp', 'c0', gains[:, :], gains_d, writes=[P.tok('gains')])
    P.dma('sp', 'c0', small[:, :], small_d, writes=[P.tok('gains')])
    P.dma('sp', 'c0', cts[:, :], cts_d, writes=[P.tok('gains')])
    P.dma('pool', 'c1', ident4[:, :, :], ident_d.rearrange("p (a b) -> p a b", a=4), writes=[P.tok('consts')])
    P.dma('pool', 'c1', cmask[:, :, :], cmask_d.rearrange("p (a b) -> p a b", a=4), writes=[P.tok('consts')])
    P.memset('dve', ones_bf[:, :], 1.0, [], [P.tok('consts2')])
    P.memset('dve', eps_t[:, :], EPS, [], [P.tok('consts2')])
    P.ts('dve', nbf[:, 0:2], small[:, 8:10], -1.0, None, ALU.mult, None, [P.tok('gains')], [P.tok('nbf')])
    CT = [P.tok('gains'), P.tok('consts'), P.tok('consts2')]

    def xtok(c, tc):
        return P.tok('x', c, tc)

    def htok(c, tc):
        return P.tok('h', c, tc)

    def ffn(l, which, gidx):
        wi = wi_d[which][l]
        wo = wo_d[which][l]
        A_OFF = 0
        actT = view(A_OFF, [11, T], BF16)
        NT_OFF = 45056
        sg_t = [view(57344 + 2048 * i, [TC], F32) for i in range(2)]
        norm_x(gidx, NT_OFF)
        wi_v = wi.rearrange("(kc p) n -> p kc n", p=128)
        wo_v = wo.rearrange("(fc p) n -> p fc n", p=128)
        sgi = 0
        for fh in range(2):
            for f in range(11):
                fg = fh * 11 + f
                wt, wtk = load_w(wi_ring, None, parts=[
                    (lambda t: t[:, :, 0:128], wi_v[:, :, fg * 128:(fg + 1) * 128]),
                    (lambda t: t[:, :, 128:256], wi_v[:, :, DFF + fg * 128: DFF + (fg + 1) * 128])])
                for tc in range(NTC):
                    ts = slice(tc * TC, (tc + 1) * TC)
                    gb, gtk, _ = psS.next()
                    ub, utk, _ = psS.next()
                    for kc in range(8):
                        mm(gb[:, :], wt[:, kc, 0:128], hT[:, kc, ts], kc == 0, kc == 7,
                           [wtk, htok(kc, tc)] + CT, [gtk], kc == 7)
                    for kc in range(8):
                        mm(ub[:, :], wt[:, kc, 128:256], hT[:, kc, ts], kc == 0, kc == 7,
                           [wtk, htok(kc, tc)], [utk], kc == 7)
                    sg = sg_t[sgi % 2]
                    sgk = P.tok('sg', sgi % 2)
                    sgi += 1
                    P.act(sg[:, :], gb[:, :], AF.Silu, [gtk], [sgk])
                    P.tt('dve', actT[:, f, ts], sg[:, :], ub[:, :], ALU.mult, [sgk, utk], [P.tok('act', f, tc)])
            for m in range(8):
                wt, wtk = load_w(wo_ring, None, parts=[
                    (lambda t: t[:, :, :], wo_v[:, fh * 11:(fh + 1) * 11, m * 128:(m + 1) * 128])])
                for tc in range(NTC):
                    ts = slice(tc * TC, (tc + 1) * TC)
                    ob, otk, _ = psA.next()
                    for f in range(11):
                        mm(ob[:, :], wt[:, f, :], actT[:, f, ts], f == 0, f == 10,
                           [wtk, P.tok('act', f, tc)], [otk], f == 10)
                    P.stt('dve', xT[:, m, ts], ob[:, :], 0.5, xT[:, m, ts], ALU.mult, ALU.add,
                          [otk, xtok(m, tc)], [xtok(m, tc)])
        P.barrier()


    def wslot(parts):
        tile, tk, st = wi_ring.next()
        for (k0, k1, c0, c1, src) in parts:
            P.dma('pool', st, tile[:, k0:k1, c0:c1], src, writes=[tk])
        return tile, tk

    def fm(w2d):
        return w2d.rearrange("(kc p) n -> p kc n", p=128)

    def proj(wt, wtk, c0, M, src_fn, nkc, tc, n=TC, ring=None):
        bank, btk, _ = (ring or psS).next()
        for kc in range(nkc):
            sap, stk = src_fn(kc)
            mm(bank[0:M, 0:n], wt[:, kc, c0:c0 + M], sap, kc == 0, kc == nkc - 1, [wtk, stk] + CT, [btk], kc == nkc - 1)
        return bank, btk

    def h_src(tc):
        ts = slice(tc * TC, (tc + 1) * TC)
        return lambda kc: (hT[:, kc, ts], htok(kc, tc))

    def rope_evac(dst, dtk, A, Atk, B, Btk, p0, p1, ts, scale, tabs, ropetmp):
        cosT, sinT = tabs
        rt = ropetmp
        P.stt('dve', rt[p0:p1, :], A[p0:p1, :], scale, cosT[p0:p1, ts], ALU.mult, ALU.mult,
              [Atk, P.tok('tabs')], [P.tok('ropetmp')])
        P.stt('dve', B[p0:p1, :], B[p0:p1, :], scale, sinT[p0:p1, ts], ALU.mult, ALU.mult,
              [Btk, P.tok('tabs')], [Btk])
        P.tt('dve', dst, rt[p0:p1, :], B[p0:p1, :], ALU.add, [P.tok('ropetmp'), Btk], [dtk])

    MX_MIXED, MX_TAB, MX_PT, MX_Q, MX_K, MX_V = 0, 32768, 40960, 44032, 48128, 52224
    MX_LAT, MX_KR, MX_NT, MX_WSC, MX_RDEN, MX_ROPE = 56320, 68608, 72704, 84992, 86016, 88064

    def attn_core(Qt, Kt, Vt, kdim, nchunks, nj_fn, causal, den_aug, qtok_fn, ktok_fn, vtok_fn, out_fn, pt_off, rden_off, extra=()):
        PTs = [view(pt_off + 1024 * i, [TC], BF16) for i in range(3)]
        rden = view(rden_off, [TC], F32)
        st = {'k': 0}

        def emit_S(i, j):
            ts = slice(i * TC, (i + 1) * TC)
            bank, btk, _ = psS.next()
            diag = causal and j >= 4 * i
            c0 = 128 * (j - 4 * i) if diag else 0
            mm(bank[:, c0:TC], Kt[0:kdim, j * 128:(j + 1) * 128], Qt[0:kdim, i * TC + c0:(i + 1) * TC], True, not diag,
               [ktok_fn(j), qtok_fn(i)] + CT + list(extra), [btk], not diag)
            if diag:
                mm(bank[:, c0:c0 + 128], ident4[:, 0, :], cmask[:, 0, 0:128], False, True, CT, [btk], True)
            return bank, btk, c0

        for i in range(nchunks):
            nj = nj_fn(i)
            acc, atk, _ = psA.next()
            if not den_aug:
                den, dtk, _ = psA.next()
            nxt = emit_S(i, 0)
            for j in range(nj):
                bank, btk, c0 = nxt
                if j + 1 < nj:
                    nxt = emit_S(i, j + 1)
                k = st['k'] % 3
                st['k'] += 1
                P.act(PTs[k][:, c0:TC], bank[:, c0:TC], AF.Exp, [btk], [P.tok('PT', k)])
                mm(acc[:, c0:TC], Vt[:, j, :], PTs[k][:, c0:TC], j == 0, j == nj - 1, [vtok_fn(j), P.tok('PT', k)] + list(extra), [atk], j == nj - 1)
                if not den_aug:
                    mm(den[:, :], ones_bf[:, :], PTs[k][:, :], j == 0, j == nj - 1, [P.tok('PT', k)], [dtk], j == nj - 1)
            if den_aug:
                P.recip('dve', rden[0:64, :], acc[64:128, :], [atk], [P.tok('rden')])
            else:
                P.recip('dve', rden[:, :], den[:, :], [dtk], [P.tok('rden')])
            out_fn(i, acc, atk, rden, P.tok('rden'))

    def vtok_evac(Vt, src_fn, wt, wtk, c0, nkc, ntiles, vtokname):
        for j0 in range(0, ntiles, 4):
            bank, btk, _ = psS.next()
            for jj in range(4):
                j = j0 + jj
                for kc in range(nkc):
                    sap, stk = src_fn(kc, j)
                    mm(bank[:, jj * 64:(jj + 1) * 64], sap, wt[:, kc, c0:c0 + 64], kc == 0, kc == nkc - 1,
                       [wtk, stk] + CT, [btk], kc == nkc - 1 and jj == 3)
            P.copy('act' if (j0 // 4) % 2 else 'dve', Vt[:, j0:j0 + 4, 0:64],
                   bank[:, 0:256].rearrange("p (a b) -> p a b", a=4), [btk], [P.tok(vtokname, j0 // 4)])

    def mixed_out_fn(mixedT, chunk, pbase):
        def f(i, acc, atk, rden, rtk):
            ts = slice(i * TC, (i + 1) * TC)
            P.tt('dve', mixedT[pbase:pbase + 64, chunk, ts], acc[0:64, :], rden[0:64, :], ALU.mult,
                 [atk, rtk], [P.tok('mixed', chunk, i)])
        return f

    def mixer(l):
        mixedT = view(MX_MIXED, [8, T], BF16)
        tabs2 = view(MX_TAB, [2, T], BF16)
        tabs = (tabs2[:, 0, :], tabs2[:, 1, :])
        Qt = view(MX_Q, [T], BF16)
        Kt = view(MX_K, [T], BF16)
        Vt = view(MX_V, [16, 128], BF16)
        ropetmp = view(MX_ROPE, [TC], F32)
        win = fm(win_d[l])
        winx = fm(winx_d[l])
        norm_x(l * 5 + 1, MX_NT)
        P.barrier()
        if cfg.get('mix') is not None:
            for c in range(8):
                P.memset('dve', mixedT[:, c, :], 0.0, [], [P.tok('mixed', c, tc) for tc in range(NTC)])
        P.memset('dve', Vt[:, :, 64:128], 1.0, [], [P.tok('Vones')])
        CTm = [P.tok('Vones')]

        def qtok(i):
            return P.tok('Q', i)

        def ktok(j):
            return P.tok('K', j // 4)

        def vtok(j):
            return P.tok('V', j // 4)

        if 'fox' in (cfg.get('mix') or {'fox', 'mla', 'dsa'}):
            ones_f = view(MX_TAB, [T], F32)
            sp_f = view(MX_NT, [T], F32)
            cum_f = view(MX_Q, [T], F32)
            c3 = view(MX_LAT, [3, T], BF16)
            wt, wtk = wslot([(0, 8, 0, 6, win[:, :, O_FF:O_FF + 6])])
            P.memset('dve', ones_f[0:6, :], 1.0, [], [P.tok('ones_f')])
            for tc in range(NTC):
                ts = slice(tc * TC, (tc + 1) * TC)
                bank, btk = proj(wt, wtk, 0, 6, h_src(tc), 8, tc)
                P.act(sp_f[0:6, ts], bank[0:6, :], AF.Exp, [btk, P.tok('nbf')], [P.tok('sp')], bias=nbf[0:6, l:l + 1], scale=-1.0)
            P.act(sp_f[0:6, :], sp_f[0:6, :], AF.Ln, [P.tok('sp')], [P.tok('sp')], bias=1.0)
            P.op('dve', lambda e: e.tensor_tensor_scan(out=cum_f[0:6, :], data0=ones_f[0:6, :], data1=sp_f[0:6, :],
                                                        initial=0.0, op0=ALU.mult, op1=ALU.subtract),
                 [P.tok('sp'), P.tok('ones_f')], [P.tok('cum')])
            P.copy('dve', c3[0:6, 0, :], cum_f[0:6, :], [P.tok('cum')], [P.tok('c3')])
            P.tt('dve', sp_f[0:6, :], cum_f[0:6, :], c3[0:6, 0, :], ALU.subtract, [P.tok('cum'), P.tok('c3')], [P.tok('sp')])
            P.copy('dve', c3[0:6, 1, :], sp_f[0:6, :], [P.tok('sp')], [P.tok('c3')])
            P.tt('dve', cum_f[0:6, :], sp_f[0:6, :], c3[0:6, 1, :], ALU.subtract, [P.tok('sp'), P.tok('c3')], [P.tok('cum')])
            P.copy('dve', c3[0:6, 2, :], cum_f[0:6, :], [P.tok('cum')], [P.tok('c3')])
            P.barrier()
            fsets = [(Qt, Kt, Vt), (view(MX_TAB, [T], BF16), view(MX_TAB + 4096, [T], BF16), view(MX_KR, [16, 128], BF16))]
            P.memset('dve', fsets[1][2][:, :, 64:128], 1.0, [], [P.tok('Vones')])
            for h in range(6):
                sid = h % 2
                Qt, Kt, Vt = fsets[sid]
                wt, wtk = wslot([(0, 8, 0, 64, win[:, :, O_FQ + h * 64:O_FQ + (h + 1) * 64]),
                                 (0, 8, 64, 128, win[:, :, O_FK + h * 64:O_FK + (h + 1) * 64]),
                                 (0, 8, 128, 192, win[:, :, O_FV + h * 64:O_FV + (h + 1) * 64])])
                P.memset('dve', Qt[64:70, :], -1.0, [], [P.tok('Qaug', sid)])
                P.memset('dve', Kt[64:70, :], 1.0, [], [P.tok('Kaug', sid)])
                for k3 in range(3):
                    P.dma('sp', 'augq%d' % sid, Qt[64 + k3:65 + k3, :], c3[h:h + 1, k3, :], reads=[P.tok('c3')],
                          writes=[P.tok('Qaug', sid)])
                    P.dma('sp', 'augk%d' % sid, Kt[67 + k3:68 + k3, :], c3[h:h + 1, k3, :], reads=[P.tok('c3')],
                          writes=[P.tok('Kaug', sid)])
                for tc in range(NTC):
                    ts = slice(tc * TC, (tc + 1) * TC)
                    bank, btk = proj(wt, wtk, 0, 64, h_src(tc), 8, tc)
                    P.act(Qt[0:64, ts], bank[0:64, :], AF.Copy, [btk], [P.tok('Q', sid, tc)], scale=0.125)
                    bank, btk = proj(wt, wtk, 64, 64, h_src(tc), 8, tc)
                    P.copy('dve', Kt[0:64, ts], bank[0:64, :], [btk], [P.tok('K', sid, tc)])
                vtok_evac(Vt, lambda kc, j: (hT[:, kc, j * 128:(j + 1) * 128], htok(kc, j // 4)), wt, wtk, 128, 8, 16,
                          'V%d' % sid)
                _attn_aug(Qt, Kt, Vt, 70, mixedT, h // 2, (h % 2) * 64,
                          extra=[P.tok('Qaug', sid), P.tok('Kaug', sid)] + CTm, sid=sid)
            Qt, Kt, Vt = fsets[0]
            P.barrier()

        if 'mla' in (cfg.get('mix') or {'fox', 'mla', 'dsa'}):
            P.dma('pool', 'tabs', tabs2[:, 0, :], tab_d[0], writes=[P.tok('tabs')])
            P.dma('pool', 'tabs', tabs2[:, 1, :], tab_d[1], writes=[P.tok('tabs')])
            cqn = view(MX_LAT, [2, T], BF16)
            ckvn = view(MX_LAT + 8192, [T], BF16)
            kr = view(MX_KR, [T], BF16)
            wtA, wtAk = wslot([(0, 8, 0, 256, win[:, :, O_CQ:O_CQ + 256])])
            wtB, wtBk = wslot([(0, 8, 0, 128, win[:, :, O_CKV:O_CKV + 128]),
                               (0, 8, 128, 160, win[:, :, O_KR:O_KR + 32]),
                               (0, 8, 160, 192, winx[:, :, X_KR:X_KR + 32])])
            for tc in range(NTC):
                ts = slice(tc * TC, (tc + 1) * TC)
                b0, b0k = proj(wtA, wtAk, 0, 128, h_src(tc), 8, tc)
                b1, b1k = proj(wtA, wtAk, 128, 128, h_src(tc), 8, tc)
                norm_chunk([(b0[:, :], b0k), (b1[:, :], b1k)], 256, lambda c: small[:, l * 2 + c:l * 2 + c + 1],
                           [(cqn[:, 0, ts], P.tok('cqn', tc)), (cqn[:, 1, ts], P.tok('cqn', tc))], TC, MX_NT)
                b2, b2k = proj(wtB, wtBk, 0, 128, h_src(tc), 8, tc)
                norm_chunk([(b2[:, :], b2k)], 128, lambda c: small[:, 4 + l:5 + l],
                           [(ckvn[:, ts], P.tok('ckvn', tc))], TC, MX_NT)
                A, Ak = proj(wtB, wtBk, 128, 32, h_src(tc), 8, tc)
                B, Bk = proj(wtB, wtBk, 160, 32, h_src(tc), 8, tc)
                rope_evac(kr[0:32, ts], P.tok('kr', tc), A, Ak, B, Bk, 0, 32, ts, 1.0, tabs, ropetmp)
            sc = 96.0 ** -0.5
            P.barrier()
            msets = [(Qt, Kt, Vt), (view(MX_NT, [T], BF16), view(MX_NT + 4096, [T], BF16), view(MX_NT + 8192, [16, 128], BF16))]
            P.memset('dve', msets[1][2][:, :, 64:128], 1.0, [], [P.tok('Vones')])
            for h in range(6):
                sid = h % 2
                Qt, Kt, Vt = msets[sid]
                wt, wtk = wslot([(0, 2, 0, 96, fm(wuq_d[l])[:, :, h * 96:(h + 1) * 96]),
                                 (0, 2, 96, 192, fm(wuqs_d[l])[:, :, h * 96:(h + 1) * 96]),
                                 (2, 3, 0, 128, fm(wukv_d[l])[:, :, h * 128:(h + 1) * 128])])
                for tc in range(NTC):
                    ts = slice(tc * TC, (tc + 1) * TC)
                    csrc = lambda kc, tc=tc, ts=ts: (cqn[:, kc, ts], P.tok('cqn', tc))
                    A, Ak = proj(wt, wtk, 0, 96, csrc, 2, tc)
                    B, Bk = proj(wt, wtk, 96, 96, csrc, 2, tc)
                    P.act(Qt[0:64, ts], A[0:64, :], AF.Copy, [Ak], [P.tok('Q', sid, tc)], scale=sc)
                    rope_evac(Qt[64:96, ts], P.tok('Q', sid, tc), A, Ak, B, Bk, 64, 96, ts, sc, tabs, ropetmp)
                    bank, btk = psS.next()[0:2]
                    mm(bank[0:64, :], wt[:, 2, 0:64], ckvn[:, ts], True, True, [wtk, P.tok('ckvn', tc)] + CT, [btk], True)
                    P.copy('dve', Kt[0:64, ts], bank[0:64, :], [btk], [P.tok('K', sid, tc)])
                    P.copy('act', Kt[64:96, ts], kr[0:32, ts], [P.tok('kr', tc)], [P.tok('K', sid, tc)])
                vtok_evac(Vt, lambda kc, j: (ckvn[:, j * 128:(j + 1) * 128], P.tok('ckvn', j // 4)), wt[:, 2:3, :], wtk,
                          64, 1, 16, 'V%d' % sid)
                _attn_aug(Qt, Kt, Vt, 96, mixedT, 3 + h // 2, (h % 2) * 64, extra=CTm, sid=sid)
            Qt, Kt, Vt = msets[0]
            P.barrier()

        if 'dsa' in (cfg.get('mix') or {'fox', 'mla', 'dsa'}):
            dsa(l, mixedT, tabs2, tabs, ropetmp, win, winx, CTm)

        wo_v = fm(wout_d[l])
        for m in range(8):
            wt, wtk = wslot([(0, 8, 0, 128, wo_v[:, :, m * 128:(m + 1) * 128])])
            for tc in range(NTC):
                ts = slice(tc * TC, (tc + 1) * TC)
                ob, otk, _ = psA.next()
                for c in range(8):
                    mm(ob[:, :], wt[:, c, 0:128], mixedT[:, c, ts], c == 0, c == 7,
                       [wtk, P.tok('mixed', c, tc)] + CT, [otk], c == 7)
                P.tt('dve', xT[:, m, ts], ob[:, :], xT[:, m, ts], ALU.add, [otk, xtok(m, tc)], [xtok(m, tc)])
        P.barrier()

    def _attn_aug(Qt, Kt, Vt, kdim, mixedT, chunk, pbase, extra, sid=0):
        attn_core(Qt, Kt, Vt, kdim, NTC, lambda i: 4 * (i + 1), True, True,
                  lambda i: P.tok('Q', sid, i), lambda j: P.tok('K', sid, j // 4), lambda j: P.tok('V%d' % sid, j // 4),
                  mixed_out_fn(mixedT, chunk, pbase), MX_PT, MX_RDEN, extra=extra)

    def dsa(l, mixedT, tabs2, tabs, ropetmp, win, winx, CTm):
        NIT = 14
        BR = 8.0
        qi = view(MX_LAT, [3, T], BF16)
        ki = view(MX_KR, [T], BF16)
        qc = view(MX_Q, [2, T], BF16)
        kc_t = view(MX_NT, [T], BF16)
        Vt = view(MX_V, [16, 128], BF16)
        wsc = view(MX_WSC, [2, 128], F32)
        P.dma('pool', 'tabs', tabs2[:, 0, :], tab_d[0], writes=[P.tok('tabs')])
        P.dma('pool', 'tabs', tabs2[:, 1, :], tab_d[1], writes=[P.tok('tabs')])
        for g in range(3):
            M = 96 if g < 2 else 64
            wt, wtk = wslot([(0, 8, 0, M, win[:, :, O_DQI + g * 96:O_DQI + g * 96 + M]),
                             (0, 8, 96, 96 + M, winx[:, :, X_DQI + g * 96:X_DQI + g * 96 + M])])
            for tc in range(NTC):
                ts = slice(tc * TC, (tc + 1) * TC)
                A, Ak = proj(wt, wtk, 0, M, h_src(tc), 8, tc)
                B, Bk = proj(wt, wtk, 96, M, h_src(tc), 8, tc)
                rope_evac(qi[0:M, g, ts], P.tok('qi', g, tc), A, Ak, B, Bk, 0, M, ts, 1.0, tabs, ropetmp)
        wt, wtk = wslot([(0, 8, 0, 96, winx[:, :, X_KI:X_KI + 96]), (0, 8, 96, 192, winx[:, :, X_KIS:X_KIS + 96]),
                         (0, 8, 192, 200, win[:, :, O_DWI:O_DWI + 8])])
        for tc in range(NTC):
            ts = slice(tc * TC, (tc + 1) * TC)
            A, Ak = proj(wt, wtk, 0, 96, h_src(tc), 8, tc)
            B, Bk = proj(wt, wtk, 96, 96, h_src(tc), 8, tc)
            rope_evac(ki[0:96, ts], P.tok('ki', tc), A, Ak, B, Bk, 0, 96, ts, 1.0, tabs, ropetmp)
        bank, btk, _ = psS.next()
        for j in range(16):
            for kc in range(8):
                mm(bank[:, j * 8:(j + 1) * 8], hT[:, kc, j * 128:(j + 1) * 128], wt[:, kc, 192:200], kc == 0, kc == 7,
                   [wtk, htok(kc, j // 4)] + CT, [btk], kc == 7 and j == 15)
        P.copy('act', wsc[:, 0, :], bank[:, 0:128], [btk], [P.tok('wsc')])
        P.barrier()
        P.dma('pool', 'tabs', tabs2[:, 0, :], tab_d[2], writes=[P.tok('tabs')])
        P.dma('pool', 'tabs', tabs2[:, 1, :], tab_d[3], writes=[P.tok('tabs')])
        for sl in range(2):
            wt, wtk = wslot([(0, 8, 0, 128, win[:, :, O_DQ + sl * 128:O_DQ + (sl + 1) * 128]),
                             (0, 8, 128, 256, winx[:, :, X_DQ + sl * 128:X_DQ + (sl + 1) * 128])])
            for tc in range(NTC):
                ts = slice(tc * TC, (tc + 1) * TC)
                A, Ak = proj(wt, wtk, 0, 128, h_src(tc), 8, tc)
                B, Bk = proj(wt, wtk, 128, 128, h_src(tc), 8, tc)
                rope_evac(qc[:, sl, ts], P.tok('qc', tc), A, Ak, B, Bk, 0, 128, ts, 0.125, tabs, ropetmp)
        wt, wtk = wslot([(0, 8, 0, 64, win[:, :, O_DK:O_DK + 64]), (0, 8, 64, 128, win[:, :, O_DK:O_DK + 64]),
                         (0, 8, 128, 192, winx[:, :, X_DK:X_DK + 64]), (0, 8, 192, 256, winx[:, :, X_DK:X_DK + 64])])
        for tc in range(NTC):
            ts = slice(tc * TC, (tc + 1) * TC)
            A, Ak = proj(wt, wtk, 0, 128, h_src(tc), 8, tc)
            B, Bk = proj(wt, wtk, 128, 128, h_src(tc), 8, tc)
            rope_evac(kc_t[:, ts], P.tok('kc', tc), A, Ak, B, Bk, 0, 128, ts, 1.0, tabs, ropetmp)
        wt, wtk = wslot([(0, 8, 0, 64, win[:, :, O_DV:O_DV + 64])])
        vtok_evac(Vt, lambda kc, j: (hT[:, kc, j * 128:(j + 1) * 128], htok(kc, j // 4)), wt, wtk, 0, 8, 16, 'V')
        P.barrier()
        hflat = hT[:, 0:8, :].rearrange("p a b -> p (a b)")
        iscs = [hflat[:, 4096 * k:4096 * (k + 1)].bitcast(F32) for k in range(2)]
        negms = [hflat[:, 8192 + 2048 * k: 8192 + 2048 * (k + 1)] for k in range(2)]
        rbufs = [hflat[:, 12288 + 512 * k: 12288 + 512 * (k + 1)] for k in range(4)]
        Dts = [hflat[:, 14336 + 1024 * k: 14336 + 1024 * (k + 1)].rearrange("p (a b) -> p a b", a=8) for k in range(2)]
        bis = _cache.get(('bis', l))
        if bis is None:
            bis = _cache[('bis', l)] = sb('bis%d' % l, [128, 8], F32)
        cnt, stp, lo = bis[:, 1:2], bis[:, 2:3], bis[:, 3:4]
        mids = [bis[:, 0:1], bis[:, 4:5]]
        PTs = [view(MX_PT + 1024 * i, [TC], BF16) for i in range(3)]
        rden = view(MX_RDEN, [TC], F32)
        ident_bf = ident4[:, 0, :]
        cts_bf = view(MX_ROPE, [128], BF16)
        P.copy('dve', cts_bf[:, :], cts[:, :], CT, [P.tok('cts_bf')])
        RSC = (8.0 ** -0.5) * (32.0 ** -0.5)
        stc = {'rk': 0, 'kk': 0}

        def stageA_prep(i):
            Dt = Dts[i % 2]
            for hh in range(8):
                P.ts('dve', Dt[:, hh, :], ident_bf, wsc[:, 0, i * 8 + hh:i * 8 + hh + 1], None, ALU.mult, None,
                     [P.tok('wsc')] + CT, [P.tok('Dt', i % 2)])

        def stageA(i):
            W = (i + 1) * 128
            tt_ = slice(i * 128, (i + 1) * 128)
            isc = iscs[i % 2]
            Dt = Dts[i % 2]
            for c0 in range(0, W, TC):
                n = min(TC, W - c0)
                ib, ibk, _ = psA.next()
                diag = (c0 + n == W)
                pend = []

                def flush_one():
                    hh2, rb2, rbk2 = pend.pop(0)
                    mm(ib[:, 0:n], Dt[:, hh2, :], rb2[:, 0:n], hh2 == 0, (hh2 == 7 and not diag), [rbk2, P.tok('Dt', i % 2)], [ibk],
                       hh2 == 7 and not diag)
                for hh in range(8):
                    g, r = hh // 3, hh % 3
                    bank, btk, _ = psS.next()
                    mm(bank[:, 0:n], qi[r * 32:(r + 1) * 32, g, tt_], ki[r * 32:(r + 1) * 32, c0:c0 + n], True, True,
                       [P.tok('qi', g, i // 4)] + [P.tok('ki', q) for q in range(c0 // TC, (c0 + n - 1) // TC + 1)] + CT,
                       [btk], True)
                    rb = rbufs[stc['rk'] % 4]
                    rbk = P.tok('rbuf', stc['rk'] % 4)
                    stc['rk'] += 1
                    P.act(rb[:, 0:n], bank[:, 0:n], AF.Relu, [btk], [rbk], scale=RSC)
                    pend.append((hh, rb, rbk))
                    if len(pend) > 2:
                        flush_one()
                while pend:
                    flush_one()
                if diag:
                    mm(ib[:, n - 128:n], ident_bf, cts_bf[:, :], False, True, [P.tok('cts_bf')], [ibk], True)
                P.copy('act', isc[:, c0:c0 + n], ib[:, 0:n], [ibk], [P.tok('isc', i % 2, c0)])

        def stageB(i):
            W = (i + 1) * 128
            tt_ = slice(i * 128, (i + 1) * 128)
            negm = negms[i % 2]
            ntk = P.tok('negm', i % 2)
            if i < 2:
                if i == 1:
                    P.memset('dve', negm[:, 0:128], 0.0, [], [ntk])
                P.ts('dve', negm[:, tt_], cts[:, :], NEG, None, ALU.max, None, CT, [ntk])
                return
            isc = iscs[i % 2]
            itoks = [P.tok('isc', i % 2, c0) for c0 in range(0, W, TC)]
            mid = mids[0]
            P.memset('dve', mid, 0.0, [], [P.tok('mid')])
            for it in range(NIT):
                d = BR / (2.0 ** it)
                P.ts('dve', negm[:, 0:W], isc[:, 0:W], mid, 0.0, ALU.is_ge, ALU.add, itoks + [P.tok('mid')],
                     [ntk, P.tok('cnt')], accum_out=cnt)
                P.ts('dve', stp, cnt, 255.5, 0.5, ALU.is_ge, ALU.subtract, [P.tok('cnt')], [P.tok('stp')])
                P.stt('dve', mid, stp, d, mid, ALU.mult, ALU.add, [P.tok('stp'), P.tok('mid')], [P.tok('mid')])
            P.ts('dve', lo, mid, -BR / (2.0 ** NIT), None, ALU.add, None, [P.tok('mid')], [P.tok('lo')])
            P.ts('dve', negm[:, 0:W], isc[:, 0:W], lo, NEG, ALU.is_lt, ALU.mult, itoks + [P.tok('lo')], [ntk])

        def stageC(i):
            tt_ = slice(i * 128, (i + 1) * 128)
            negm = negms[i % 2]
            ntk = P.tok('negm', i % 2)
            acc, atk, _ = psA.next()

            def emit_S(j):
                bank, btk, _ = psS.next()
                for p in range(2):
                    mm(bank[:, p * 256:(p + 1) * 256], kc_t[p * 64:(p + 1) * 64, j * 128:(j + 1) * 128],
                       qc[p * 64:(p + 1) * 64, :, tt_], True, False,
                       [P.tok('kc', j // 4), P.tok('qc', i // 4)] + CT, [btk], False)
                    mm(bank[:, p * 256:(p + 1) * 256], negm[:, j * 128:(j + 1) * 128], ident4[:, 0:2, :], False, True,
                       [ntk], [btk], p == 1)
                return bank, btk
            nxt = emit_S(0)
            for j in range(i + 1):
                bank, btk = nxt
                if j < i:
                    nxt = emit_S(j + 1)
                k = stc['kk'] % 3
                stc['kk'] += 1
                P.act(PTs[k][:, :], bank[:, :], AF.Exp, [btk], [P.tok('PT', k)])
                mm(acc[:, :], Vt[:, j, :], PTs[k][:, :], j == 0, j == i, [P.tok('V', j // 4), P.tok('PT', k)] + CTm,
                   [atk], j == i)
            stc['acc', i] = (acc, atk)

        def stageC_norm(i):
            tt_ = slice(i * 128, (i + 1) * 128)
            acc, atk = stc.pop(('acc', i))
            P.recip('dve', rden[0:64, :], acc[64:128, :], [atk], [P.tok('rden')])
            for cb in range(4):
                p, sl = cb // 2, cb % 2
                P.tt('dve', mixedT[p * 64:(p + 1) * 64, 6 + sl, tt_], acc[0:64, cb * 128:(cb + 1) * 128],
                     rden[0:64, cb * 128:(cb + 1) * 128], ALU.mult, [atk, P.tok('rden')], [P.tok('mixed', 6 + sl, i // 4)])

        stageA_prep(2)
        stageA(2)
        for i in range(16):
            nxtA = (i + 1) if 3 <= i + 1 <= 15 else None
            if nxtA is not None:
                stageA_prep(nxtA)
            stageB(i)
            if i >= 1:
                stageC_norm(i - 1)
            if nxtA is not None:
                stageA(nxtA)
            stageC(i)
        stageC_norm(15)
        P.barrier()

    _cache = {}

    def xattn(l, s):
        oxT = view(0, [4, T], BF16)
        memT = view(16384, [8, MEM], F32)
        hm = view(24576, [8, MEM], BF16)
        XNT = 28672
        Qt = view(MX_Q, [T], BF16)
        Kt = view(MX_K, [MEM], BF16)
        Vt = view(MX_V, [2, 128], BF16)
        norm_x(l * 5 + 2, XNT)
        for c in range(8):
            P.dma('sp', 'memld%d' % c, memT[:, c, :], memT_d[s, c * 128:(c + 1) * 128, :], writes=[P.tok('memT', c)])
        norm_chunk([(memT[:, c, :], P.tok('memT', c)) for c in range(8)], D, lambda c: gain_ap(l * 5 + 3, c),
                   [(hm[:, c, :], P.tok('hm', c)) for c in range(8)], MEM, XNT)
        sc = 128.0 ** -0.5
        for h in range(4):
            wq, wqk = wslot([(0, 8, 0, 128, fm(xwq_d[l])[:, :, h * 128:(h + 1) * 128])])
            wkv, wkvk = wslot([(0, 8, 0, 128, fm(xwkv_d[l])[:, :, h * 128:(h + 1) * 128]),
                               (0, 8, 128, 256, fm(xwkv_d[l])[:, :, 512 + h * 128:512 + (h + 1) * 128])])
            for tc in range(NTC):
                ts = slice(tc * TC, (tc + 1) * TC)
                bank, btk = proj(wq, wqk, 0, 128, h_src(tc), 8, tc)
                P.act(Qt[:, ts], bank[:, :], AF.Copy, [btk], [P.tok('Q', tc)], scale=sc)
            bank, btk = proj(wkv, wkvk, 0, 128, lambda kc: (hm[:, kc, :], P.tok('hm', kc)), 8, 0, n=MEM)
            P.copy('dve', Kt[:, :], bank[:, 0:MEM], [btk], [P.tok('K', 0)])
            bank, btk, _ = psS.next()
            for j in range(2):
                for kc in range(8):
                    mm(bank[:, j * 128:(j + 1) * 128], hm[:, kc, j * 128:(j + 1) * 128], wkv[:, kc, 128:256], kc == 0, kc == 7,
                       [wkvk, P.tok('hm', kc)] + CT, [btk], kc == 7 and j == 1)
            P.copy('dve', Vt[:, :, :], bank[:, 0:256].rearrange("p (a b) -> p a b", a=2), [btk], [P.tok('V', 0)])

            def out_fn(i, acc, atk, rden, rtk, h=h):
                ts = slice(i * TC, (i + 1) * TC)
                P.tt('dve', oxT[:, h, ts], acc[:, :], rden[:, :], ALU.mult, [atk, rtk], [P.tok('ox', h, i)])
            attn_core(Qt, Kt, Vt, 128, NTC, lambda i: 2, False, False, lambda i: P.tok('Q', i),
                      lambda j: P.tok('K', 0), lambda j: P.tok('V', 0), out_fn, MX_PT, MX_RDEN)
        xo = xwo_d[l].rearrange("(hc p) n -> p hc n", p=128)
        for m in range(8):
            wt, wtk = wslot([(0, 4, 0, 128, xo[:, :, m * 128:(m + 1) * 128])])
            for tc in range(NTC):
                ts = slice(tc * TC, (tc + 1) * TC)
                ob, otk, _ = psA.next()
                for h in range(4):
                    mm(ob[:, :], wt[:, h, 0:128], oxT[:, h, ts], h == 0, h == 3, [wtk, P.tok('ox', h, tc)] + CT, [otk], h == 3)
                P.tt('dve', xT[:, m, ts], ob[:, :], xT[:, m, ts], ALU.add, [otk, xtok(m, tc)], [xtok(m, tc)])
        P.barrier()

    def final_norm(s):
        NT_OFF = 0
        ob = [view(16384 + 2048 * i, [TC], F32) for i in range(4)]
        k = [0]

        def dst_fn(c, t0, n):
            return ob[k[0] % 4][:, 0:n]

        sq = view(NT_OFF, [8, TC], BF16)
        s_t = view(NT_OFF + 8192, [TC], F32)
        r_t = view(NT_OFF + 10240, [TC], F32)
        for tc in range(NTC):
            t0 = tc * TC
            for c in range(8):
                P.act(sq[:, c, :], xT[:, c, t0:t0 + TC], AF.Square, [xtok(c, tc)], [P.tok('nt_sq', c)])
            bank, btk, _ = psM.next()
            for c in range(8):
                mm(bank[:, :], ones_bf[:, :], sq[:, c, :], c == 0, c == 7, [P.tok('nt_sq', c)] + CT, [btk], c == 7)
            P.act(s_t[:, :], bank[:, :], AF.Sqrt, [btk], [P.tok('nt_s')], bias=eps_t[:, 0:1], scale=1.0 / D)
            P.recip('dve', r_t[:, :], s_t[:, :], [P.tok('nt_s')], [P.tok('nt_r')])
            for c in range(8):
                o = ob[k[0] % 4]
                otk = P.tok('fo', k[0] % 4)
                ost = 'outst%d' % (k[0] % 4)
                k[0] += 1
                P.stt('dve', o[:, :], xT[:, c, t0:t0 + TC], gain_ap(10, c), r_t[:, :], ALU.mult, ALU.mult,
                      [xtok(c, tc), P.tok('nt_r')], [otk])
                P.dma('sp', ost, out_d[s, c * 128:(c + 1) * 128, t0:t0 + TC], o[:, :], reads=[otk])
        P.barrier()

    for s in range(nseq):
        for c in range(8):
            P.dma('sp', 'xload%d' % c, xT[:, c, :], xT_d[s, c * 128:(c + 1) * 128, :],
                  writes=[xtok(c, tc) for tc in range(NTC)])
        for l in range(layers):
            if 'ffn1' in phases:
                ffn(l, 0, l * 5 + 0)
            if 'mix' in phases:
                mixer(l)
            if 'xa' in phases:
                xattn(l, s)
            if 'ffn2' in phases:
                ffn(l, 1, l * 5 + 4)
        if 'final' in phases:
            final_norm(s)
        else:
            for c in range(8):
                P.dma('sp', 'outst%d' % (c % 4), out_d[s, c * 128:(c + 1) * 128, :], xT[:, c, :],
                      reads=[xtok(c, tc) for tc in range(NTC)])
    for st_name, v in P.dcnt.items():
        if st_name.startswith('outst') and v > 0:
            P.need('sp', (st_name, v), True)
    P.emit()
    return es, P


def _fm(g):
    return np.ascontiguousarray(g.reshape(8, 128).T)


def host_prep(inputs, nseq_total=None):
    f = {k: np.asarray(v, dtype=np.float32) for k, v in inputs.items()}
    shared = {}
    for k in ['ffn1_wi', 'ffn2_wi', 'ffn1_wo', 'ffn2_wo', 'w_in', 'mla_w_uq', 'mla_w_ukv', 'w_out',
              'xa_wq', 'xa_wkv', 'xa_wo']:
        shared[k] = np.ascontiguousarray(f[k])
    w_in = f['w_in']
    def swap_idx(base, nheads, d):
        h = d // 2
        idx = []
        for hh in range(nheads):
            b = base + hh * d
            idx += list(range(b + h, b + d)) + list(range(b, b + h))
        return idx
    cols = (swap_idx(O_KR, 1, 32) + swap_idx(O_DQ, 4, 64) + swap_idx(O_DK, 1, 64) + swap_idx(O_DQI, 8, 32)
            + list(range(O_DKI, O_DKI + 32)) * 3 + swap_idx(O_DKI, 1, 32) * 3)
    assert len(cols) == NX
    shared['w_inx'] = np.ascontiguousarray(w_in[:, :, cols])
    uq_cols = []
    for hh in range(6):
        b = hh * 96
        uq_cols += list(range(b, b + 64)) + list(range(b + 80, b + 96)) + list(range(b + 64, b + 80))
    shared['mla_w_uqs'] = np.ascontiguousarray(f['mla_w_uq'][:, :, uq_cols])
    gains = np.zeros((128, 96), np.float32)
    for l in range(L):
        for i, k in enumerate(['ffn1_norm', 'mix_norm', 'xa_norm', 'mem_norm', 'ffn2_norm']):
            gains[:, (l * 5 + i) * 8:(l * 5 + i + 1) * 8] = _fm(f[k][l])
    gains[:, 80:88] = _fm(f['final_norm'])
    shared['gains'] = gains
    small = np.zeros((128, 16), np.float32)
    for l in range(L):
        small[:, l * 2:(l + 1) * 2] = f['mla_q_norm'][l].reshape(2, 128).T
        small[:, 4 + l] = f['mla_kv_norm'][l]
        small[0:6, 8 + l] = f['b_forget'][l]
    shared['small'] = small
    pos = np.arange(T, dtype=np.float32)
    p = np.arange(128)
    tabs = np.zeros((4, 128, T), np.float32)
    for ti, half in enumerate([16, 32]):
        inv = (10000.0 ** (-(np.arange(half, dtype=np.float32)) / half)).astype(np.float32)
        ang = pos[None, :] * inv[p % half][:, None]
        sign = np.where((p % (2 * half)) < half, -1.0, 1.0).astype(np.float32)
        tabs[2 * ti] = np.cos(ang)
        tabs[2 * ti + 1] = np.sin(ang) * sign[:, None]
    shared['tables'] = tabs
    sl = np.arange(128)[:, None]
    tl = np.arange(TC)[None, :]
    cm = np.zeros((128, 4, TC), np.float32)
    for r in range(4):
        cm[:, r, :] = np.where(tl >= sl + 128 * r, 0.0, NEG)
    shared['cmask'] = cm.reshape(128, 4 * TC)
    shared['ident4'] = np.tile(np.eye(128, dtype=np.float32), (1, 4))
    shared['causal_ts'] = np.where(np.arange(128)[None, :] <= np.arange(128)[:, None], 0.0, BIGNEG).astype(np.float32)
    xT = np.ascontiguousarray(np.transpose(f['x'], (0, 2, 1)))
    memT = np.ascontiguousarray(np.transpose(f['mem'], (0, 2, 1)))
    return shared, xT, memT


def kernel(**inputs):
    shared, xT, memT = host_prep(inputs)
    ncore = 8
    nc = bass.Bass("TRN2", target_bir_lowering=False)
    es, P = build(nc, {})
    in_maps = []
    for c in range(ncore):
        m = dict(shared)
        m['xT'] = xT[c * NSEQ:(c + 1) * NSEQ]
        m['memT'] = memT[c * NSEQ:(c + 1) * NSEQ]
        in_maps.append(m)
    res = run_bass_kernel_spmd(nc, in_maps, core_ids=list(range(ncore)))
    es.close()
    outT = np.concatenate([r['outT'] for r in res.results], axis=0)
    return np.ascontiguousarray(np.transpose(outT, (0, 2, 1))).astype(np.float32)
```

```python
import numpy as np
from contextlib import ExitStack
import concourse.bass as bass
import concourse.mybir as mybir
from concourse.bass_utils import run_bass_kernel_spmd

F32 = mybir.dt.float32
BF16 = mybir.dt.bfloat16
AF = mybir.ActivationFunctionType
ALU = mybir.AluOpType

D = 1024
T = 2048
L = 2
NSEQ = 2
DFF = 2816
NF = 22
MEM = 256
TC = 512
NTC = T // TC
EPS = 1e-6
NEG = -30000.0
BIGNEG = -1.0e30
ENG = ['pe', 'act', 'dve', 'pool', 'sp']

O_FQ, O_FK, O_FV, O_FF = 0, 384, 768, 1152
O_CQ, O_CKV, O_KR = 1158, 1414, 1542
O_DQ, O_DK, O_DV, O_DQI, O_DKI, O_DWI = 1574, 1830, 1894, 1958, 2214, 2246
X_KR, X_DQ, X_DK, X_DQI, X_KI, X_KIS = 0, 32, 288, 352, 608, 704
NX = 800


class Tok:
    __slots__ = ('w', 'r')

    def __init__(self):
        self.w = None
        self.r = {}


class Prog:
    def __init__(self, nc, es):
        self.nc = nc
        self.es = es
        self.semh = {e: es.enter_context(nc.semaphore('s_' + e)) for e in ENG}
        self.cnt = {e: 0 for e in ENG}
        self.seen = {e: {} for e in ENG}
        self.q = {e: [] for e in ENG}
        self.clock = {e: [None] for e in ENG}
        self.toks = {}
        self.dcnt = {}
        self.nwait = 0

    def tok(self, *key):
        t = self.toks.get(key)
        if t is None:
            t = self.toks[key] = Tok()
        return t

    def stream(self, name):
        if name not in self.semh:
            self.semh[name] = self.es.enter_context(self.nc.semaphore('d_' + name))
            self.dcnt[name] = 0
        return name

    def need(self, eng, ev, raw):
        if ev is None:
            return
        key, val = ev
        if key == eng and (eng == 'pe' or eng == 'sp' or not raw):
            return
        sn = self.seen[eng]
        if sn.get(key, 0) >= val:
            return
        sem = self.semh[key]
        self.q[eng].append(lambda e, sem=sem, val=val: e.wait_ge(sem, val))
        self.nwait += 1
        sn[key] = val
        if key in self.clock and val < len(self.clock[key]):
            for k2, v2 in self.clock[key][val].items():
                if sn.get(k2, 0) < v2:
                    sn[k2] = v2

    def _deps(self, eng, reads, writes):
        for t in reads:
            self.need(eng, t.w, True)
        for t in writes:
            self.need(eng, t.w, False)
            for k, v in t.r.items():
                self.need(eng, (k, v), False)

    def _mark(self, ev, reads, writes):
        k, v = ev
        for t in reads:
            if t.r.get(k, 0) < v:
                t.r[k] = v
        for t in writes:
            t.w = ev
            t.r = {}

    def op(self, eng, fn, reads=(), writes=(), inc=True):
        self._deps(eng, reads, writes)
        if inc:
            self.cnt[eng] += 1
            ev = (eng, self.cnt[eng])
            sem = self.semh[eng]
            self.q[eng].append(lambda e, fn=fn, sem=sem: fn(e).then_inc(sem, 1))
            self.clock[eng].append(dict(self.seen[eng]))
        else:
            ev = (eng, self.cnt[eng] + 1)
            self.q[eng].append(lambda e, fn=fn: fn(e))
        self._mark(ev, reads, writes)

    def dma(self, eng, stream, out, in_, reads=(), writes=()):
        self.stream(stream)
        self._deps(eng, reads, writes)
        self.dcnt[stream] += 16
        ev = (stream, self.dcnt[stream])
        sem = self.semh[stream]
        self.q[eng].append(lambda e, out=out, in_=in_, sem=sem: e.dma_start(out=out, in_=in_).then_inc(sem, 16))
        self._mark(ev, reads, writes)
        return ev

    def act(self, out, in_, func, reads, writes, bias=None, scale=None):
        kw = {}
        if bias is not None:
            kw['bias'] = bias
        if scale is not None:
            kw['scale'] = scale
        self.op('act', lambda e: e.activation(out=out, in_=in_, func=func, **kw), reads, writes)

    def stt(self, eng, out, in0, scalar, in1, op0, op1, reads, writes):
        self.op(eng, lambda e: e.scalar_tensor_tensor(out=out, in0=in0, scalar=scalar, in1=in1, op0=op0, op1=op1),
                reads, writes)

    def tt(self, eng, out, in0, in1, op, reads, writes):
        self.op(eng, lambda e: e.tensor_tensor(out=out, in0=in0, in1=in1, op=op), reads, writes)

    def ts(self, eng, out, in0, s1, s2, op0, op1, reads, writes, accum_out=None):
        kw = {}
        if accum_out is not None:
            kw['accum_out'] = accum_out
        if op1 is None:
            self.op(eng, lambda e: e.tensor_scalar(out=out, in0=in0, scalar1=s1, scalar2=None, op0=op0, **kw),
                    reads, writes)
        else:
            self.op(eng, lambda e: e.tensor_scalar(out=out, in0=in0, scalar1=s1, scalar2=s2, op0=op0, op1=op1, **kw),
                    reads, writes)

    def recip(self, eng, out, in_, reads, writes):
        self.op(eng, lambda e: e.reciprocal(out=out, in_=in_), reads, writes)

    def copy(self, eng, out, in_, reads, writes):
        if eng == 'act':
            self.op(eng, lambda e: e.copy(out=out, in_=in_), reads, writes)
        else:
            self.op(eng, lambda e: e.tensor_copy(out=out, in_=in_), reads, writes)

    def memset(self, eng, out, val, reads, writes):
        self.op(eng, lambda e: e.memset(out, val), reads, writes)

    def barrier(self):
        for e in ['pe', 'act', 'dve', 'pool', 'sp']:
            for o in ['pe', 'act', 'dve', 'pool']:
                if o != e and self.cnt[o] > 0:
                    self.need(e, (o, self.cnt[o]), True)
            for st, v in self.dcnt.items():
                if v > 0 and not (st.startswith('wi') or st.startswith('wo')):
                    self.need(e, (st, v), True)

    def emit(self):
        nc = self.nc
        q = self.q
        with nc.Block() as block:
            @block.tensor
            def _(e):
                for f in q['pe']:
                    f(e)

            @block.scalar
            def _(e):
                for f in q['act']:
                    f(e)

            @block.vector
            def _(e):
                for f in q['dve']:
                    f(e)

            @block.gpsimd
            def _(e):
                for f in q['pool']:
                    f(e)

            @block.sync
            def _(e):
                for f in q['sp']:
                    f(e)


class Ring:
    def __init__(self, P, name, tiles):
        self.P = P
        self.name = name
        self.tiles = tiles
        self.i = 0

    def next(self):
        k = self.i % len(self.tiles)
        self.i += 1
        return self.tiles[k], self.P.tok(self.name, k), '%s%d' % (self.name, k)


def build(nc, cfg):
    nseq = cfg.get('nseq', NSEQ)
    layers = cfg.get('layers', L)
    phases = cfg.get('phases', {'ffn1', 'mix', 'xa', 'ffn2', 'final'})
    es = ExitStack()
    P = Prog(nc, es)

    used_inputs = cfg.get('inputs', None)

    def din(name, shape, dt=F32):
        if used_inputs is not None and name not in used_inputs:
            return None
        return nc.dram_tensor(name, list(shape), dt, kind="ExternalInput").ap()

    xT_d = din('xT', [nseq, D, T])
    memT_d = din('memT', [nseq, D, MEM])
    wi_d = [din('ffn1_wi', [L, D, 2 * DFF]), din('ffn2_wi', [L, D, 2 * DFF])]
    wo_d = [din('ffn1_wo', [L, DFF, D]), din('ffn2_wo', [L, DFF, D])]
    win_d = din('w_in', [L, D, 2254])
    winx_d = din('w_inx', [L, D, NX])
    wuq_d = din('mla_w_uq', [L, 256, 576])
    wuqs_d = din('mla_w_uqs', [L, 256, 576])
    wukv_d = din('mla_w_ukv', [L, 128, 768])
    wout_d = din('w_out', [L, D, D])
    xwq_d = din('xa_wq', [L, D, 512])
    xwkv_d = din('xa_wkv', [L, D, 1024])
    xwo_d = din('xa_wo', [L, 512, D])
    gains_d = din('gains', [128, 96])
    small_d = din('small', [128, 16])
    tab_d = din('tables', [4, 128, T])
    cmask_d = din('cmask', [128, 4 * TC])
    ident_d = din('ident4', [128, 4 * 128])
    cts_d = din('causal_ts', [128, 128])
    out_d = nc.dram_tensor('outT', [nseq, D, T], F32, kind="ExternalOutput").ap()

    def sb(name, shape, dt):
        return es.enter_context(nc.sbuf_tensor('sb_' + name, list(shape), dt))

    def ps(name):
        return es.enter_context(nc.psum_tensor(name, [128, TC], F32))

    xT = sb('xT', [128, 8, T], F32)
    hT = sb('hT', [128, 8, T], BF16)
    ident4 = sb('ident4', [128, 4, 128], BF16)
    cmask = sb('cmask', [128, 4, TC], BF16)
    ones_bf = sb('ones_bf', [128, 128], BF16)
    cts = sb('cts', [128, 128], F32)
    gains = sb('gains', [128, 96], F32)
    small = sb('small', [128, 16], F32)
    nbf = sb('nbf', [128, 2], F32)
    wi_tiles = [sb('wiring%d' % i, [128, 8, 256], BF16) for i in range(3)]
    wo_tiles = [sb('woring%d' % i, [128, 11, 128], BF16) for i in range(2)]
    wi_ring = Ring(P, 'wi', wi_tiles)
    wo_ring = Ring(P, 'wo', wo_tiles)
    SCR_BYTES = 90112
    scr = sb('scr', [128, SCR_BYTES // 4], F32)

    def view(off, shape, dt):
        n = int(np.prod(shape))
        esz = 4 if dt == F32 else 2
        assert off % 4 == 0 and (n * esz) % 4 == 0 and off + n * esz <= SCR_BYTES, (off, shape)
        a = scr[:, off // 4: off // 4 + (n * esz) // 4]
        if dt != F32:
            a = a.bitcast(dt)
        if len(shape) == 2:
            a = a.rearrange("p (a b) -> p a b", a=shape[0])
        elif len(shape) == 3:
            a = a.rearrange("p (a b c) -> p a b c", a=shape[0], b=shape[1])
        return a

    banks = [ps('bank%d' % i) for i in range(8)]
    psS = Ring(P, 'psS', banks[0:4])
    psA = Ring(P, 'psA', banks[4:7])
    psM = Ring(P, 'psM', banks[7:8])

    def mm(out, lhsT, rhs, start, stop, reads, writes, inc):
        P.op('pe', lambda e: e.matmul(out, lhsT, rhs, start=start, stop=stop), reads=reads, writes=writes, inc=inc)

    def load_w(ring, dram_ap, ncols_total=None, parts=None):
        tile, tk, st = ring.next()
        for i, (dst_fn, src) in enumerate(parts):
            P.dma('pool', st, dst_fn(tile), src, writes=[tk])
        return tile, tk

    def gain_ap(idx, c):
        return gains[:, idx * 8 + c: idx * 8 + c + 1]

    def norm_chunk(srcs, nfeat, gain_fn, dsts, n, nt_off):
        sq = view(nt_off, [8, TC], BF16)
        s_t = view(nt_off + 8192, [TC], F32)
        r_t = view(nt_off + 10240, [TC], F32)
        nch = len(srcs)
        for c, (sap, stk) in enumerate(srcs):
            P.act(sq[:, c, 0:n], sap, AF.Square, [stk], [P.tok('nt_sq', c)])
        bank, btk, _ = psM.next()
        for c in range(nch):
            mm(bank[:, 0:n], ones_bf[:, :], sq[:, c, 0:n], c == 0, c == nch - 1,
               [P.tok('nt_sq', c)] + CT, [btk], c == nch - 1)
        P.act(s_t[:, 0:n], bank[:, 0:n], AF.Sqrt, [btk], [P.tok('nt_s')], bias=eps_t[:, 0:1], scale=1.0 / nfeat)
        P.recip('dve', r_t[:, 0:n], s_t[:, 0:n], [P.tok('nt_s')], [P.tok('nt_r')])
        for c, ((sap, stk), (dap, dtk)) in enumerate(zip(srcs, dsts)):
            P.stt('dve', dap, sap, gain_fn(c), r_t[:, 0:n], ALU.mult, ALU.mult, [stk, P.tok('nt_r')], [dtk])

    def norm_x(gidx, nt_off):
        for tc in range(NTC):
            ts = slice(tc * TC, (tc + 1) * TC)
            norm_chunk([(xT[:, c, ts], xtok(c, tc)) for c in range(8)], D, lambda c: gain_ap(gidx, c),
                       [(hT[:, c, ts], htok(c, tc)) for c in range(8)], TC, nt_off)

    eps_t = sb('eps_t', [128, 1], F32)

    P.dma('sp', 'c0', gains[:, :], gains_d, writes=[P.tok('gains')])
    P.dma('sp', 'c0', small[:, :], small_d, writes=[P.tok('gains')])
    P.dma('sp', 'c0', cts[:, :], cts_d, writes=[P.tok('gains')])
    P.dma('pool', 'c1', ident4[:, :, :], ident_d.rearrange("p (a b) -> p a b", a=4), writes=[P.tok('consts')])
    P.dma('pool', 'c1', cmask[:, :, :], cmask_d.rearrange("p (a b) -> p a b", a=4), writes=[P.tok('consts')])
    P.memset('dve', ones_bf[:, :], 1.0, [], [P.tok('consts2')])
    P.memset('dve', eps_t[:, :], EPS, [], [P.tok('consts2')])
    P.ts('dve', nbf[:, 0:2], small[:, 8:10], -1.0, None, ALU.mult, None, [P.tok('gains')], [P.tok('nbf')])
    CT = [P.tok('gains'), P.tok('consts'), P.tok('consts2')]

    def xtok(c, tc):
        return P.tok('x', c, tc)

    def htok(c, tc):
        return P.tok('h', c, tc)

    def ffn(l, which, gidx):
        wi = wi_d[which][l]
        wo = wo_d[which][l]
        A_OFF = 0
        actT = view(A_OFF, [11, T], BF16)
        NT_OFF = 45056
        sg_t = [view(57344 + 2048 * i, [TC], F32) for i in range(2)]
        norm_x(gidx, NT_OFF)
        wi_v = wi.rearrange("(kc p) n -> p kc n", p=128)
        wo_v = wo.rearrange("(fc p) n -> p fc n", p=128)
        sgi = 0
        for fh in range(2):
            for f in range(11):
                fg = fh * 11 + f
                wt, wtk = load_w(wi_ring, None, parts=[
                    (lambda t: t[:, :, 0:128], wi_v[:, :, fg * 128:(fg + 1) * 128]),
                    (lambda t: t[:, :, 128:256], wi_v[:, :, DFF + fg * 128: DFF + (fg + 1) * 128])])
                for tc in range(NTC):
                    ts = slice(tc * TC, (tc + 1) * TC)
                    gb, gtk, _ = psS.next()
                    ub, utk, _ = psS.next()
                    for kc in range(8):
                        mm(gb[:, :], wt[:, kc, 0:128], hT[:, kc, ts], kc == 0, kc == 7,
                           [wtk, htok(kc, tc)] + CT, [gtk], kc == 7)
                    for kc in range(8):
                        mm(ub[:, :], wt[:, kc, 128:256], hT[:, kc, ts], kc == 0, kc == 7,
                           [wtk, htok(kc, tc)], [utk], kc == 7)
                    sg = sg_t[sgi % 2]
                    sgk = P.tok('sg', sgi % 2)
                    sgi += 1
                    P.act(sg[:, :], gb[:, :], AF.Silu, [gtk], [sgk])
                    P.tt('dve', actT[:, f, ts], sg[:, :], ub[:, :], ALU.mult, [sgk, utk], [P.tok('act', f, tc)])
            for m in range(8):
                wt, wtk = load_w(wo_ring, None, parts=[
                    (lambda t: t[:, :, :], wo_v[:, fh * 11:(fh + 1) * 11, m * 128:(m + 1) * 128])])
                for tc in range(NTC):
                    ts = slice(tc * TC, (tc + 1) * TC)
                    ob, otk, _ = psA.next()
                    for f in range(11):
                        mm(ob[:, :], wt[:, f, :], actT[:, f, ts], f == 0, f == 10,
                           [wtk, P.tok('act', f, tc)], [otk], f == 10)
                    P.stt('dve', xT[:, m, ts], ob[:, :], 0.5, xT[:, m, ts], ALU.mult, ALU.add,
                          [otk, xtok(m, tc)], [xtok(m, tc)])
        P.barrier()


    def wslot(parts):
        tile, tk, st = wi_ring.next()
        for (k0, k1, c0, c1, src) in parts:
            P.dma('pool', st, tile[:, k0:k1, c0:c1], src, writes=[tk])
        return tile, tk

    def fm(w2d):
        return w2d.rearrange("(kc p) n -> p kc n", p=128)

    def proj(wt, wtk, c0, M, src_fn, nkc, tc, n=TC, ring=None):
        bank, btk, _ = (ring or psS).next()
        for kc in range(nkc):
            sap, stk = src_fn(kc)
            mm(bank[0:M, 0:n], wt[:, kc, c0:c0 + M], sap, kc == 0, kc == nkc - 1, [wtk, stk] + CT, [btk], kc == nkc - 1)
        return bank, btk

    def h_src(tc):
        ts = slice(tc * TC, (tc + 1) * TC)
        return lambda kc: (hT[:, kc, ts], htok(kc, tc))

    def rope_evac(dst, dtk, A, Atk, B, Btk, p0, p1, ts, scale, tabs, ropetmp):
        cosT, sinT = tabs
        rt = ropetmp
        P.stt('dve', rt[p0:p1, :], A[p0:p1, :], scale, cosT[p0:p1, ts], ALU.mult, ALU.mult,
              [Atk, P.tok('tabs')], [P.tok('ropetmp')])
        P.stt('dve', B[p0:p1, :], B[p0:p1, :], scale, sinT[p0:p1, ts], ALU.mult, ALU.mult,
              [Btk, P.tok('tabs')], [Btk])
        P.tt('dve', dst, rt[p0:p1, :], B[p0:p1, :], ALU.add, [P.tok('ropetmp'), Btk], [dtk])

    MX_MIXED, MX_TAB, MX_PT, MX_Q, MX_K, MX_V = 0, 32768, 40960, 44032, 48128, 52224
    MX_LAT, MX_KR, MX_NT, MX_WSC, MX_RDEN, MX_ROPE = 56320, 68608, 72704, 84992, 86016, 88064

    def attn_core(Qt, Kt, Vt, kdim, nchunks, nj_fn, causal, den_aug, qtok_fn, ktok_fn, vtok_fn, out_fn, pt_off, rden_off, extra=()):
        PTs = [view(pt_off + 1024 * i, [TC], BF16) for i in range(3)]
        rden = view(rden_off, [TC], F32)
        st = {'k': 0}

        def emit_S(i, j):
            ts = slice(i * TC, (i + 1) * TC)
            bank, btk, _ = psS.next()
            diag = causal and j >= 4 * i
            c0 = 128 * (j - 4 * i) if diag else 0
            mm(bank[:, c0:TC], Kt[0:kdim, j * 128:(j + 1) * 128], Qt[0:kdim, i * TC + c0:(i + 1) * TC], True, not diag,
               [ktok_fn(j), qtok_fn(i)] + CT + list(extra), [btk], not diag)
            if diag:
                mm(bank[:, c0:c0 + 128], ident4[:, 0, :], cmask[:, 0, 0:128], False, True, CT, [btk], True)
            return bank, btk, c0

        for i in range(nchunks):
            nj = nj_fn(i)
            acc, atk, _ = psA.next()
            if not den_aug:
                den, dtk, _ = psA.next()
            LA = 2
            pendS = [emit_S(i, jj) for jj in range(min(LA, nj))]
            for j in range(nj):
                bank, btk, c0 = pendS.pop(0)
                if j + LA < nj:
                    pendS.append(emit_S(i, j + LA))
                k = st['k'] % 3
                st['k'] += 1
                P.act(PTs[k][:, c0:TC], bank[:, c0:TC], AF.Exp, [btk], [P.tok('PT', k)])
                mm(acc[:, c0:TC], Vt[:, j, :], PTs[k][:, c0:TC], j == 0, j == nj - 1, [vtok_fn(j), P.tok('PT', k)] + list(extra), [atk], j == nj - 1)
                if not den_aug:
                    mm(den[:, :], ones_bf[:, :], PTs[k][:, :], j == 0, j == nj - 1, [P.tok('PT', k)], [dtk], j == nj - 1)
            if den_aug:
                P.recip('dve', rden[0:64, :], acc[64:128, :], [atk], [P.tok('rden')])
            else:
                P.recip('dve', rden[:, :], den[:, :], [dtk], [P.tok('rden')])
            out_fn(i, acc, atk, rden, P.tok('rden'))

    def vtok_evac(Vt, src_fn, wt, wtk, c0, nkc, ntiles, vtokname):
        for j0 in range(0, ntiles, 4):
            bank, btk, _ = psS.next()
            for jj in range(4):
                j = j0 + jj
                for kc in range(nkc):
                    sap, stk = src_fn(kc, j)
                    mm(bank[:, jj * 64:(jj + 1) * 64], sap, wt[:, kc, c0:c0 + 64], kc == 0, kc == nkc - 1,
                       [wtk, stk] + CT, [btk], kc == nkc - 1 and jj == 3)
            P.copy('act' if (j0 // 4) % 2 else 'dve', Vt[:, j0:j0 + 4, 0:64],
                   bank[:, 0:256].rearrange("p (a b) -> p a b", a=4), [btk], [P.tok(vtokname, j0 // 4)])

    def mixed_out_fn(mixedT, chunk, pbase):
        def f(i, acc, atk, rden, rtk):
            ts = slice(i * TC, (i + 1) * TC)
            P.tt('dve', mixedT[pbase:pbase + 64, chunk, ts], acc[0:64, :], rden[0:64, :], ALU.mult,
                 [atk, rtk], [P.tok('mixed', chunk, i)])
        return f

    def mixer(l):
        mixedT = view(MX_MIXED, [8, T], BF16)
        tabs2 = view(MX_TAB, [2, T], BF16)
        tabs = (tabs2[:, 0, :], tabs2[:, 1, :])
        Qt = view(MX_Q, [T], BF16)
        Kt = view(MX_K, [T], BF16)
        Vt = view(MX_V, [16, 128], BF16)
        ropetmp = view(MX_ROPE, [TC], F32)
        win = fm(win_d[l])
        winx = fm(winx_d[l])
        norm_x(l * 5 + 1, MX_NT)
        P.barrier()
        if cfg.get('mix') is not None:
            for c in range(8):
                P.memset('dve', mixedT[:, c, :], 0.0, [], [P.tok('mixed', c, tc) for tc in range(NTC)])
        P.memset('dve', Vt[:, :, 64:128], 1.0, [], [P.tok('Vones')])
        CTm = [P.tok('Vones')]

        def qtok(i):
            return P.tok('Q', i)

        def ktok(j):
            return P.tok('K', j // 4)

        def vtok(j):
            return P.tok('V', j // 4)

        if 'fox' in (cfg.get('mix') or {'fox', 'mla', 'dsa'}):
            ones_f = view(MX_TAB, [T], F32)
            sp_f = view(MX_NT, [T], F32)
            cum_f = view(MX_Q, [T], F32)
            c3 = view(MX_LAT, [3, T], BF16)
            wt, wtk = wslot([(0, 8, 0, 6, win[:, :, O_FF:O_FF + 6])])
            P.memset('dve', ones_f[0:6, :], 1.0, [], [P.tok('ones_f')])
            for tc in range(NTC):
                ts = slice(tc * TC, (tc + 1) * TC)
                bank, btk = proj(wt, wtk, 0, 6, h_src(tc), 8, tc)
                P.act(sp_f[0:6, ts], bank[0:6, :], AF.Exp, [btk, P.tok('nbf')], [P.tok('sp')], bias=nbf[0:6, l:l + 1], scale=-1.0)
            P.act(sp_f[0:6, :], sp_f[0:6, :], AF.Ln, [P.tok('sp')], [P.tok('sp')], bias=1.0)
            P.op('dve', lambda e: e.tensor_tensor_scan(out=cum_f[0:6, :], data0=ones_f[0:6, :], data1=sp_f[0:6, :],
                                                        initial=0.0, op0=ALU.mult, op1=ALU.subtract),
                 [P.tok('sp'), P.tok('ones_f')], [P.tok('cum')])
            P.copy('dve', c3[0:6, 0, :], cum_f[0:6, :], [P.tok('cum')], [P.tok('c3')])
            P.tt('dve', sp_f[0:6, :], cum_f[0:6, :], c3[0:6, 0, :], ALU.subtract, [P.tok('cum'), P.tok('c3')], [P.tok('sp')])
            P.copy('dve', c3[0:6, 1, :], sp_f[0:6, :], [P.tok('sp')], [P.tok('c3')])
            P.tt('dve', cum_f[0:6, :], sp_f[0:6, :], c3[0:6, 1, :], ALU.subtract, [P.tok('sp'), P.tok('c3')], [P.tok('cum')])
            P.copy('dve', c3[0:6, 2, :], cum_f[0:6, :], [P.tok('cum')], [P.tok('c3')])
            P.barrier()
            for h in range(6):
                wt, wtk = wslot([(0, 8, 0, 64, win[:, :, O_FQ + h * 64:O_FQ + (h + 1) * 64]),
                                 (0, 8, 64, 128, win[:, :, O_FK + h * 64:O_FK + (h + 1) * 64]),
                                 (0, 8, 128, 192, win[:, :, O_FV + h * 64:O_FV + (h + 1) * 64])])
                P.memset('dve', Qt[64:70, :], -1.0, [], [P.tok('Qaug')])
                P.memset('dve', Kt[64:70, :], 1.0, [], [P.tok('Kaug')])
                for k3 in range(3):
                    P.dma('sp', 'augq', Qt[64 + k3:65 + k3, :], c3[h:h + 1, k3, :], reads=[P.tok('c3')], writes=[P.tok('Qaug')])
                    P.dma('sp', 'augk', Kt[67 + k3:68 + k3, :], c3[h:h + 1, k3, :], reads=[P.tok('c3')], writes=[P.tok('Kaug')])
                for tc in range(NTC):
                    ts = slice(tc * TC, (tc + 1) * TC)
                    bank, btk = proj(wt, wtk, 0, 64, h_src(tc), 8, tc)
                    P.act(Qt[0:64, ts], bank[0:64, :], AF.Copy, [btk], [P.tok('Q', tc)], scale=0.125)
                    bank, btk = proj(wt, wtk, 64, 64, h_src(tc), 8, tc)
                    P.copy('dve', Kt[0:64, ts], bank[0:64, :], [btk], [P.tok('K', tc)])
                vtok_evac(Vt, lambda kc, j: (hT[:, kc, j * 128:(j + 1) * 128], htok(kc, j // 4)), wt, wtk, 128, 8, 16, 'V')
                _attn_aug(Qt, Kt, Vt, 70, mixedT, h // 2, (h % 2) * 64, extra=[P.tok('Qaug'), P.tok('Kaug')] + CTm)
            P.barrier()

        if 'mla' in (cfg.get('mix') or {'fox', 'mla', 'dsa'}):
            P.dma('pool', 'tabs', tabs2[:, 0, :], tab_d[0], writes=[P.tok('tabs')])
            P.dma('pool', 'tabs', tabs2[:, 1, :], tab_d[1], writes=[P.tok('tabs')])
            cqn = view(MX_LAT, [2, T], BF16)
            ckvn = view(MX_LAT + 8192, [T], BF16)
            kr = view(MX_KR, [T], BF16)
            wtA, wtAk = wslot([(0, 8, 0, 256, win[:, :, O_CQ:O_CQ + 256])])
            wtB, wtBk = wslot([(0, 8, 0, 128, win[:, :, O_CKV:O_CKV + 128]),
                               (0, 8, 128, 160, win[:, :, O_KR:O_KR + 32]),
                               (0, 8, 160, 192, winx[:, :, X_KR:X_KR + 32])])
            for tc in range(NTC):
                ts = slice(tc * TC, (tc + 1) * TC)
                b0, b0k = proj(wtA, wtAk, 0, 128, h_src(tc), 8, tc)
                b1, b1k = proj(wtA, wtAk, 128, 128, h_src(tc), 8, tc)
                norm_chunk([(b0[:, :], b0k), (b1[:, :], b1k)], 256, lambda c: small[:, l * 2 + c:l * 2 + c + 1],
                           [(cqn[:, 0, ts], P.tok('cqn', tc)), (cqn[:, 1, ts], P.tok('cqn', tc))], TC, MX_NT)
                b2, b2k = proj(wtB, wtBk, 0, 128, h_src(tc), 8, tc)
                norm_chunk([(b2[:, :], b2k)], 128, lambda c: small[:, 4 + l:5 + l],
                           [(ckvn[:, ts], P.tok('ckvn', tc))], TC, MX_NT)
                A, Ak = proj(wtB, wtBk, 128, 32, h_src(tc), 8, tc)
                B, Bk = proj(wtB, wtBk, 160, 32, h_src(tc), 8, tc)
                rope_evac(kr[0:32, ts], P.tok('kr', tc), A, Ak, B, Bk, 0, 32, ts, 1.0, tabs, ropetmp)
            sc = 96.0 ** -0.5
            for h in range(6):
                wt, wtk = wslot([(0, 2, 0, 96, fm(wuq_d[l])[:, :, h * 96:(h + 1) * 96]),
                                 (0, 2, 96, 192, fm(wuqs_d[l])[:, :, h * 96:(h + 1) * 96]),
                                 (2, 3, 0, 128, fm(wukv_d[l])[:, :, h * 128:(h + 1) * 128])])
                for tc in range(NTC):
                    ts = slice(tc * TC, (tc + 1) * TC)
                    csrc = lambda kc, tc=tc, ts=ts: (cqn[:, kc, ts], P.tok('cqn', tc))
                    A, Ak = proj(wt, wtk, 0, 96, csrc, 2, tc)
                    B, Bk = proj(wt, wtk, 96, 96, csrc, 2, tc)
                    P.act(Qt[0:64, ts], A[0:64, :], AF.Copy, [Ak], [P.tok('Q', tc)], scale=sc)
                    rope_evac(Qt[64:96, ts], P.tok('Q', tc), A, Ak, B, Bk, 64, 96, ts, sc, tabs, ropetmp)
                    bank, btk = psS.next()[0:2]
                    mm(bank[0:64, :], wt[:, 2, 0:64], ckvn[:, ts], True, True, [wtk, P.tok('ckvn', tc)] + CT, [btk], True)
                    P.copy('dve', Kt[0:64, ts], bank[0:64, :], [btk], [P.tok('K', tc)])
                    P.copy('act', Kt[64:96, ts], kr[0:32, ts], [P.tok('kr', tc)], [P.tok('K', tc)])
                vtok_evac(Vt, lambda kc, j: (ckvn[:, j * 128:(j + 1) * 128], P.tok('ckvn', j // 4)), wt[:, 2:3, :], wtk,
                          64, 1, 16, 'V')
                _attn_aug(Qt, Kt, Vt, 96, mixedT, 3 + h // 2, (h % 2) * 64, extra=CTm)
            P.barrier()

        if 'dsa' in (cfg.get('mix') or {'fox', 'mla', 'dsa'}):
            dsa(l, mixedT, tabs2, tabs, ropetmp, win, winx, CTm)

        wo_v = fm(wout_d[l])
        for m in range(8):
            wt, wtk = wslot([(0, 8, 0, 128, wo_v[:, :, m * 128:(m + 1) * 128])])
            for tc in range(NTC):
                ts = slice(tc * TC, (tc + 1) * TC)
                ob, otk, _ = psA.next()
                for c in range(8):
                    mm(ob[:, :], wt[:, c, 0:128], mixedT[:, c, ts], c == 0, c == 7,
                       [wtk, P.tok('mixed', c, tc)] + CT, [otk], c == 7)
                P.tt('dve', xT[:, m, ts], ob[:, :], xT[:, m, ts], ALU.add, [otk, xtok(m, tc)], [xtok(m, tc)])
        P.barrier()

    def _attn_aug(Qt, Kt, Vt, kdim, mixedT, chunk, pbase, extra):
        attn_core(Qt, Kt, Vt, kdim, NTC, lambda i: 4 * (i + 1), True, True,
                  lambda i: P.tok('Q', i), lambda j: P.tok('K', j // 4), lambda j: P.tok('V', j // 4),
                  mixed_out_fn(mixedT, chunk, pbase), MX_PT, MX_RDEN, extra=extra)

    def dsa(l, mixedT, tabs2, tabs, ropetmp, win, winx, CTm):
        NIT = 14
        BR = 8.0
        qi = view(MX_LAT, [3, T], BF16)
        ki = view(MX_KR, [T], BF16)
        qc = view(MX_Q, [2, T], BF16)
        kc_t = view(MX_NT, [T], BF16)
        Vt = view(MX_V, [16, 128], BF16)
        wsc = view(MX_WSC, [2, 128], F32)
        P.dma('pool', 'tabs', tabs2[:, 0, :], tab_d[0], writes=[P.tok('tabs')])
        P.dma('pool', 'tabs', tabs2[:, 1, :], tab_d[1], writes=[P.tok('tabs')])
        for g in range(3):
            M = 96 if g < 2 else 64
            wt, wtk = wslot([(0, 8, 0, M, win[:, :, O_DQI + g * 96:O_DQI + g * 96 + M]),
                             (0, 8, 96, 96 + M, winx[:, :, X_DQI + g * 96:X_DQI + g * 96 + M])])
            for tc in range(NTC):
                ts = slice(tc * TC, (tc + 1) * TC)
                A, Ak = proj(wt, wtk, 0, M, h_src(tc), 8, tc)
                B, Bk = proj(wt, wtk, 96, M, h_src(tc), 8, tc)
                rope_evac(qi[0:M, g, ts], P.tok('qi', g, tc), A, Ak, B, Bk, 0, M, ts, 1.0, tabs, ropetmp)
        wt, wtk = wslot([(0, 8, 0, 96, winx[:, :, X_KI:X_KI + 96]), (0, 8, 96, 192, winx[:, :, X_KIS:X_KIS + 96]),
                         (0, 8, 192, 200, win[:, :, O_DWI:O_DWI + 8])])
        for tc in range(NTC):
            ts = slice(tc * TC, (tc + 1) * TC)
            A, Ak = proj(wt, wtk, 0, 96, h_src(tc), 8, tc)
            B, Bk = proj(wt, wtk, 96, 96, h_src(tc), 8, tc)
            rope_evac(ki[0:96, ts], P.tok('ki', tc), A, Ak, B, Bk, 0, 96, ts, 1.0, tabs, ropetmp)
        bank, btk, _ = psS.next()
        for j in range(16):
            for kc in range(8):
                mm(bank[:, j * 8:(j + 1) * 8], hT[:, kc, j * 128:(j + 1) * 128], wt[:, kc, 192:200], kc == 0, kc == 7,
                   [wtk, htok(kc, j // 4)] + CT, [btk], kc == 7 and j == 15)
        P.copy('act', wsc[:, 0, :], bank[:, 0:128], [btk], [P.tok('wsc')])
        P.barrier()
        P.dma('pool', 'tabs', tabs2[:, 0, :], tab_d[2], writes=[P.tok('tabs')])
        P.dma('pool', 'tabs', tabs2[:, 1, :], tab_d[3], writes=[P.tok('tabs')])
        for sl in range(2):
            wt, wtk = wslot([(0, 8, 0, 128, win[:, :, O_DQ + sl * 128:O_DQ + (sl + 1) * 128]),
                             (0, 8, 128, 256, winx[:, :, X_DQ + sl * 128:X_DQ + (sl + 1) * 128])])
            for tc in range(NTC):
                ts = slice(tc * TC, (tc + 1) * TC)
                A, Ak = proj(wt, wtk, 0, 128, h_src(tc), 8, tc)
                B, Bk = proj(wt, wtk, 128, 128, h_src(tc), 8, tc)
                rope_evac(qc[:, sl, ts], P.tok('qc', tc), A, Ak, B, Bk, 0, 128, ts, 0.125, tabs, ropetmp)
        wt, wtk = wslot([(0, 8, 0, 64, win[:, :, O_DK:O_DK + 64]), (0, 8, 64, 128, win[:, :, O_DK:O_DK + 64]),
                         (0, 8, 128, 192, winx[:, :, X_DK:X_DK + 64]), (0, 8, 192, 256, winx[:, :, X_DK:X_DK + 64])])
        for tc in range(NTC):
            ts = slice(tc * TC, (tc + 1) * TC)
            A, Ak = proj(wt, wtk, 0, 128, h_src(tc), 8, tc)
            B, Bk = proj(wt, wtk, 128, 128, h_src(tc), 8, tc)
            rope_evac(kc_t[:, ts], P.tok('kc', tc), A, Ak, B, Bk, 0, 128, ts, 1.0, tabs, ropetmp)
        wt, wtk = wslot([(0, 8, 0, 64, win[:, :, O_DV:O_DV + 64])])
        vtok_evac(Vt, lambda kc, j: (hT[:, kc, j * 128:(j + 1) * 128], htok(kc, j // 4)), wt, wtk, 0, 8, 16, 'V')
        P.barrier()
        hflat = hT[:, 0:8, :].rearrange("p a b -> p (a b)")
        iscs = [hflat[:, 4096 * k:4096 * (k + 1)].bitcast(F32) for k in range(2)]
        negms = [hflat[:, 8192 + 2048 * k: 8192 + 2048 * (k + 1)] for k in range(2)]
        rbufs = [hflat[:, 12288 + 512 * k: 12288 + 512 * (k + 1)] for k in range(4)]
        Dts = [hflat[:, 14336 + 1024 * k: 14336 + 1024 * (k + 1)].rearrange("p (a b) -> p a b", a=8) for k in range(2)]
        bis = _cache.get(('bis', l))
        if bis is None:
            bis = _cache[('bis', l)] = sb('bis%d' % l, [128, 8], F32)
        cnt, stp, lo = bis[:, 1:2], bis[:, 2:3], bis[:, 3:4]
        mids = [bis[:, 0:1], bis[:, 4:5]]
        PTs = [view(MX_PT + 1024 * i, [TC], BF16) for i in range(3)]
        rden = view(MX_RDEN, [TC], F32)
        ident_bf = ident4[:, 0, :]
        cts_bf = view(MX_ROPE, [128], BF16)
        P.copy('dve', cts_bf[:, :], cts[:, :], CT, [P.tok('cts_bf')])
        RSC = (8.0 ** -0.5) * (32.0 ** -0.5)
        stc = {'rk': 0, 'kk': 0}

        def stageA_prep(i):
            Dt = Dts[i % 2]
            for hh in range(8):
                P.ts('dve', Dt[:, hh, :], ident_bf, wsc[:, 0, i * 8 + hh:i * 8 + hh + 1], None, ALU.mult, None,
                     [P.tok('wsc')] + CT, [P.tok('Dt', i % 2)])

        def stageA(i):
            W = (i + 1) * 128
            tt_ = slice(i * 128, (i + 1) * 128)
            isc = iscs[i % 2]
            Dt = Dts[i % 2]
            for c0 in range(0, W, TC):
                n = min(TC, W - c0)
                ib, ibk, _ = psA.next()
                diag = (c0 + n == W)
                pend = []

                def flush_one():
                    hh2, rb2, rbk2 = pend.pop(0)
                    mm(ib[:, 0:n], Dt[:, hh2, :], rb2[:, 0:n], hh2 == 0, (hh2 == 7 and not diag), [rbk2, P.tok('Dt', i % 2)], [ibk],
                       hh2 == 7 and not diag)
                for hh in range(8):
                    g, r = hh // 3, hh % 3
                    bank, btk, _ = psS.next()
                    mm(bank[:, 0:n], qi[r * 32:(r + 1) * 32, g, tt_], ki[r * 32:(r + 1) * 32, c0:c0 + n], True, True,
                       [P.tok('qi', g, i // 4)] + [P.tok('ki', q) for q in range(c0 // TC, (c0 + n - 1) // TC + 1)] + CT,
                       [btk], True)
                    rb = rbufs[stc['rk'] % 4]
                    rbk = P.tok('rbuf', stc['rk'] % 4)
                    stc['rk'] += 1
                    P.act(rb[:, 0:n], bank[:, 0:n], AF.Relu, [btk], [rbk], scale=RSC)
                    pend.append((hh, rb, rbk))
                    if len(pend) > 2:
                        flush_one()
                while pend:
                    flush_one()
                if diag:
                    mm(ib[:, n - 128:n], ident_bf, cts_bf[:, :], False, True, [P.tok('cts_bf')], [ibk], True)
                P.copy('act', isc[:, c0:c0 + n], ib[:, 0:n], [ibk], [P.tok('isc', i % 2, c0)])

        def stageB(i):
            W = (i + 1) * 128
            tt_ = slice(i * 128, (i + 1) * 128)
            negm = negms[i % 2]
            ntk = P.tok('negm', i % 2)
            if i < 2:
                if i == 1:
                    P.memset('dve', negm[:, 0:128], 0.0, [], [ntk])
                P.ts('dve', negm[:, tt_], cts[:, :], NEG, None, ALU.max, None, CT, [ntk])
                return
            isc = iscs[i % 2]
            itoks = [P.tok('isc', i % 2, c0) for c0 in range(0, W, TC)]
            mid = mids[0]
            P.memset('dve', mid, 0.0, [], [P.tok('mid')])
            for it in range(NIT):
                d = BR / (2.0 ** it)
                P.ts('dve', negm[:, 0:W], isc[:, 0:W], mid, 0.0, ALU.is_ge, ALU.add, itoks + [P.tok('mid')],
                     [ntk, P.tok('cnt')], accum_out=cnt)
                P.ts('dve', stp, cnt, 255.5, 0.5, ALU.is_ge, ALU.subtract, [P.tok('cnt')], [P.tok('stp')])
                P.stt('dve', mid, stp, d, mid, ALU.mult, ALU.add, [P.tok('stp'), P.tok('mid')], [P.tok('mid')])
            P.ts('dve', lo, mid, -BR / (2.0 ** NIT), None, ALU.add, None, [P.tok('mid')], [P.tok('lo')])
            P.ts('dve', negm[:, 0:W], isc[:, 0:W], lo, NEG, ALU.is_lt, ALU.mult, itoks + [P.tok('lo')], [ntk])

        def stageC(i):
            tt_ = slice(i * 128, (i + 1) * 128)
            negm = negms[i % 2]
            ntk = P.tok('negm', i % 2)
            acc, atk, _ = psA.next()

            def emit_S(j):
                bank, btk, _ = psS.next()
                for p in range(2):
                    mm(bank[:, p * 256:(p + 1) * 256], kc_t[p * 64:(p + 1) * 64, j * 128:(j + 1) * 128],
                       qc[p * 64:(p + 1) * 64, :, tt_], True, False,
                       [P.tok('kc', j // 4), P.tok('qc', i // 4)] + CT, [btk], False)
                    mm(bank[:, p * 256:(p + 1) * 256], negm[:, j * 128:(j + 1) * 128], ident4[:, 0:2, :], False, True,
                       [ntk], [btk], p == 1)
                return bank, btk
            pendS = [emit_S(jj) for jj in range(min(2, i + 1))]
            for j in range(i + 1):
                bank, btk = pendS.pop(0)
                if j + 2 <= i:
                    pendS.append(emit_S(j + 2))
                k = stc['kk'] % 3
                stc['kk'] += 1
                P.act(PTs[k][:, :], bank[:, :], AF.Exp, [btk], [P.tok('PT', k)])
                mm(acc[:, :], Vt[:, j, :], PTs[k][:, :], j == 0, j == i, [P.tok('V', j // 4), P.tok('PT', k)] + CTm,
                   [atk], j == i)
            stc['acc', i] = (acc, atk)

        def stageC_norm(i):
            tt_ = slice(i * 128, (i + 1) * 128)
            acc, atk = stc.pop(('acc', i))
            P.recip('dve', rden[0:64, :], acc[64:128, :], [atk], [P.tok('rden')])
            for cb in range(4):
                p, sl = cb // 2, cb % 2
                P.tt('dve', mixedT[p * 64:(p + 1) * 64, 6 + sl, tt_], acc[0:64, cb * 128:(cb + 1) * 128],
                     rden[0:64, cb * 128:(cb + 1) * 128], ALU.mult, [atk, P.tok('rden')], [P.tok('mixed', 6 + sl, i // 4)])

        stageA_prep(2)
        stageA(2)
        for i in range(16):
            nxtA = (i + 1) if 3 <= i + 1 <= 15 else None
            if nxtA is not None:
                stageA_prep(nxtA)
            stageB(i)
            if i >= 1:
                stageC_norm(i - 1)
            if nxtA is not None:
                stageA(nxtA)
            stageC(i)
        stageC_norm(15)
        P.barrier()

    _cache = {}

    def xattn(l, s):
        oxT = view(0, [4, T], BF16)
        memT = view(16384, [8, MEM], F32)
        hm = view(24576, [8, MEM], BF16)
        XNT = 28672
        Qt = view(MX_Q, [T], BF16)
        Kt = view(MX_K, [MEM], BF16)
        Vt = view(MX_V, [2, 128], BF16)
        norm_x(l * 5 + 2, XNT)
        for c in range(8):
            P.dma('sp', 'memld%d' % c, memT[:, c, :], memT_d[s, c * 128:(c + 1) * 128, :], writes=[P.tok('memT', c)])
        norm_chunk([(memT[:, c, :], P.tok('memT', c)) for c in range(8)], D, lambda c: gain_ap(l * 5 + 3, c),
                   [(hm[:, c, :], P.tok('hm', c)) for c in range(8)], MEM, XNT)
        sc = 128.0 ** -0.5
        for h in range(4):
            wq, wqk = wslot([(0, 8, 0, 128, fm(xwq_d[l])[:, :, h * 128:(h + 1) * 128])])
            wkv, wkvk = wslot([(0, 8, 0, 128, fm(xwkv_d[l])[:, :, h * 128:(h + 1) * 128]),
                               (0, 8, 128, 256, fm(xwkv_d[l])[:, :, 512 + h * 128:512 + (h + 1) * 128])])
            for tc in range(NTC):
                ts = slice(tc * TC, (tc + 1) * TC)
                bank, btk = proj(wq, wqk, 0, 128, h_src(tc), 8, tc)
                P.act(Qt[:, ts], bank[:, :], AF.Copy, [btk], [P.tok('Q', tc)], scale=sc)
            bank, btk = proj(wkv, wkvk, 0, 128, lambda kc: (hm[:, kc, :], P.tok('hm', kc)), 8, 0, n=MEM)
            P.copy('dve', Kt[:, :], bank[:, 0:MEM], [btk], [P.tok('K', 0)])
            bank, btk, _ = psS.next()
            for j in range(2):
                for kc in range(8):
                    mm(bank[:, j * 128:(j + 1) * 128], hm[:, kc, j * 128:(j + 1) * 128], wkv[:, kc, 128:256], kc == 0, kc == 7,
                       [wkvk, P.tok('hm', kc)] + CT, [btk], kc == 7 and j == 1)
            P.copy('dve', Vt[:, :, :], bank[:, 0:256].rearrange("p (a b) -> p a b", a=2), [btk], [P.tok('V', 0)])

            def out_fn(i, acc, atk, rden, rtk, h=h):
                ts = slice(i * TC, (i + 1) * TC)
                P.tt('dve', oxT[:, h, ts], acc[:, :], rden[:, :], ALU.mult, [atk, rtk], [P.tok('ox', h, i)])
            attn_core(Qt, Kt, Vt, 128, NTC, lambda i: 2, False, False, lambda i: P.tok('Q', i),
                      lambda j: P.tok('K', 0), lambda j: P.tok('V', 0), out_fn, MX_PT, MX_RDEN)
        xo = xwo_d[l].rearrange("(hc p) n -> p hc n", p=128)
        for m in range(8):
            wt, wtk = wslot([(0, 4, 0, 128, xo[:, :, m * 128:(m + 1) * 128])])
            for tc in range(NTC):
                ts = slice(tc * TC, (tc + 1) * TC)
                ob, otk, _ = psA.next()
                for h in range(4):
                    mm(ob[:, :], wt[:, h, 0:128], oxT[:, h, ts], h == 0, h == 3, [wtk, P.tok('ox', h, tc)] + CT, [otk], h == 3)
                P.tt('dve', xT[:, m, ts], ob[:, :], xT[:, m, ts], ALU.add, [otk, xtok(m, tc)], [xtok(m, tc)])
        P.barrier()

    def final_norm(s):
        NT_OFF = 0
        ob = [view(16384 + 2048 * i, [TC], F32) for i in range(4)]
        k = [0]

        def dst_fn(c, t0, n):
            return ob[k[0] % 4][:, 0:n]

        sq = view(NT_OFF, [8, TC], BF16)
        s_t = view(NT_OFF + 8192, [TC], F32)
        r_t = view(NT_OFF + 10240, [TC], F32)
        for tc in range(NTC):
            t0 = tc * TC
            for c in range(8):
                P.act(sq[:, c, :], xT[:, c, t0:t0 + TC], AF.Square, [xtok(c, tc)], [P.tok('nt_sq', c)])
            bank, btk, _ = psM.next()
            for c in range(8):
                mm(bank[:, :], ones_bf[:, :], sq[:, c, :], c == 0, c == 7, [P.tok('nt_sq', c)] + CT, [btk], c == 7)
            P.act(s_t[:, :], bank[:, :], AF.Sqrt, [btk], [P.tok('nt_s')], bias=eps_t[:, 0:1], scale=1.0 / D)
            P.recip('dve', r_t[:, :], s_t[:, :], [P.tok('nt_s')], [P.tok('nt_r')])
            for c in range(8):
                o = ob[k[0] % 4]
                otk = P.tok('fo', k[0] % 4)
                ost = 'outst%d' % (k[0] % 4)
                k[0] += 1
                P.stt('dve', o[:, :], xT[:, c, t0:t0 + TC], gain_ap(10, c), r_t[:, :], ALU.mult, ALU.mult,
                      [xtok(c, tc), P.tok('nt_r')], [otk])
                P.dma('sp', ost, out_d[s, c * 128:(c + 1) * 128, t0:t0 + TC], o[:, :], reads=[otk])
        P.barrier()

    for s in range(nseq):
        for c in range(8):
            P.dma('sp', 'xload%d' % c, xT[:, c, :], xT_d[s, c * 128:(c + 1) * 128, :],
                  writes=[xtok(c, tc) for tc in range(NTC)])
        for l in range(layers):
            if 'ffn1' in phases:
                ffn(l, 0, l * 5 + 0)
            if 'mix' in phases:
                mixer(l)
            if 'xa' in phases:
                xattn(l, s)
            if 'ffn2' in phases:
                ffn(l, 1, l * 5 + 4)
        if 'final' in phases:
            final_norm(s)
        else:
            for c in range(8):
                P.dma('sp', 'outst%d' % (c % 4), out_d[s, c * 128:(c + 1) * 128, :], xT[:, c, :],
                      reads=[xtok(c, tc) for tc in range(NTC)])
    for st_name, v in P.dcnt.items():
        if st_name.startswith('outst') and v > 0:
            P.need('sp', (st_name, v), True)
    P.emit()
    return es, P


def _fm(g):
    return np.ascontiguousarray(g.reshape(8, 128).T)


def host_prep(inputs, nseq_total=None):
    f = {k: np.asarray(v, dtype=np.float32) for k, v in inputs.items()}
    shared = {}
    for k in ['ffn1_wi', 'ffn2_wi', 'ffn1_wo', 'ffn2_wo', 'w_in', 'mla_w_uq', 'mla_w_ukv', 'w_out',
              'xa_wq', 'xa_wkv', 'xa_wo']:
        shared[k] = np.ascontiguousarray(f[k])
    w_in = f['w_in']
    def swap_idx(base, nheads, d):
        h = d // 2
        idx = []
        for hh in range(nheads):
            b = base + hh * d
            idx += list(range(b + h, b + d)) + list(range(b, b + h))
        return idx
    cols = (swap_idx(O_KR, 1, 32) + swap_idx(O_DQ, 4, 64) + swap_idx(O_DK, 1, 64) + swap_idx(O_DQI, 8, 32)
            + list(range(O_DKI, O_DKI + 32)) * 3 + swap_idx(O_DKI, 1, 32) * 3)
    assert len(cols) == NX
    shared['w_inx'] = np.ascontiguousarray(w_in[:, :, cols])
    uq_cols = []
    for hh in range(6):
        b = hh * 96
        uq_cols += list(range(b, b + 64)) + list(range(b + 80, b + 96)) + list(range(b + 64, b + 80))
    shared['mla_w_uqs'] = np.ascontiguousarray(f['mla_w_uq'][:, :, uq_cols])
    gains = np.zeros((128, 96), np.float32)
    for l in range(L):
        for i, k in enumerate(['ffn1_norm', 'mix_norm', 'xa_norm', 'mem_norm', 'ffn2_norm']):
            gains[:, (l * 5 + i) * 8:(l * 5 + i + 1) * 8] = _fm(f[k][l])
    gains[:, 80:88] = _fm(f['final_norm'])
    shared['gains'] = gains
    small = np.zeros((128, 16), np.float32)
    for l in range(L):
        small[:, l * 2:(l + 1) * 2] = f['mla_q_norm'][l].reshape(2, 128).T
        small[:, 4 + l] = f['mla_kv_norm'][l]
        small[0:6, 8 + l] = f['b_forget'][l]
    shared['small'] = small
    pos = np.arange(T, dtype=np.float32)
    p = np.arange(128)
    tabs = np.zeros((4, 128, T), np.float32)
    for ti, half in enumerate([16, 32]):
        inv = (10000.0 ** (-(np.arange(half, dtype=np.float32)) / half)).astype(np.float32)
        ang = pos[None, :] * inv[p % half][:, None]
        sign = np.where((p % (2 * half)) < half, -1.0, 1.0).astype(np.float32)
        tabs[2 * ti] = np.cos(ang)
        tabs[2 * ti + 1] = np.sin(ang) * sign[:, None]
    shared['tables'] = tabs
    sl = np.arange(128)[:, None]
    tl = np.arange(TC)[None, :]
    cm = np.zeros((128, 4, TC), np.float32)
    for r in range(4):
        cm[:, r, :] = np.where(tl >= sl + 128 * r, 0.0, NEG)
    shared['cmask'] = cm.reshape(128, 4 * TC)
    shared['ident4'] = np.tile(np.eye(128, dtype=np.float32), (1, 4))
    shared['causal_ts'] = np.where(np.arange(128)[None, :] <= np.arange(128)[:, None], 0.0, BIGNEG).astype(np.float32)
    xT = np.ascontiguousarray(np.transpose(f['x'], (0, 2, 1)))
    memT = np.ascontiguousarray(np.transpose(f['mem'], (0, 2, 1)))
    return shared, xT, memT


def kernel(**inputs):
    shared, xT, memT = host_prep(inputs)
    ncore = 8
    nc = bass.Bass("TRN2", target_bir_lowering=False)
    es, P = build(nc, {})
    in_maps = []
    for c in range(ncore):
        m = dict(shared)
        m['xT'] = xT[c * NSEQ:(c + 1) * NSEQ]
        m['memT'] = memT[c * NSEQ:(c + 1) * NSEQ]
        in_maps.append(m)
    res = run_bass_kernel_spmd(nc, in_maps, core_ids=list(range(ncore)))
    es.close()
    outT = np.concatenate([r['outT'] for r in res.results], axis=0)
    return np.ascontiguousarray(np.transpose(outT, (0, 2, 1))).astype(np.float32)
```
